# Optimizing a Trainium2 kernel written in Bass

```python
import math
import jax, jax.numpy as jnp
from jax import lax
import numpy as np

D_MODEL = 1024
BATCH = 8
SEQ = 8192
DEPTH = 4

CHUNK = 64
N_MIXERS = 3
NORM_EPS = 1e-6
N_SUBLAYER_NORMS = 6

HG_EXPAND = 128
HG_HEADS = D_MODEL // HG_EXPAND
HG_F_DIM = HG_HEADS * HG_EXPAND
HG_V_DIM = D_MODEL // HG_HEADS
HG_BLOCK = 16
HG_MID = HG_BLOCK // 2

DA_HEAD_DIM = 64
DA_HEADS = D_MODEL // (2 * DA_HEAD_DIM)
DA_Q_BLOCK = 128

POOL_WINDOWS = (2, 4, 8, 16)
POOL_GROUPS = len(POOL_WINDOWS)
POOL_GROUP_DIM = D_MODEL // POOL_GROUPS

D_FF = 2816

N_A = (DEPTH + 2) // 3
N_B = (DEPTH + 1) // 3
N_C = DEPTH // 3

kernel_name = "hybrid_hgrn2_diffattn_pool_macaron"


def rmsnorm(x, w):
    xf = x.astype(jnp.float32)
    y = xf * lax.rsqrt(jnp.mean(xf * xf, axis=-1, keepdims=True) + NORM_EPS)
    return (y * w.astype(jnp.float32)).astype(x.dtype)


def swiglu(x, w_in, w_out):
    g, u = jnp.split(x @ w_in, 2, axis=-1)
    return (jax.nn.silu(g) * u) @ w_out


def alibi_slopes(n_heads):
    return jnp.exp2(-8.0 * jnp.arange(1, n_heads + 1, dtype=jnp.float32) / n_heads)


def hgrn_lower_bound(lb_raw, layer):
    p = jax.nn.softmax(lb_raw.astype(jnp.float32), axis=0)
    return jnp.cumsum(p, axis=0)[layer] - p[0]


def hgrn2_mixer(x, w_in, gnorm_w, w_out, lb):
    f32 = jnp.float32
    B, S, _ = x.shape
    nb = S // HG_BLOCK
    q, f_pre, i_in, g = jnp.split(
        x @ w_in, [HG_F_DIM, 2 * HG_F_DIM, 2 * HG_F_DIM + D_MODEL], axis=-1)
    f = lb + (1.0 - lb) * jax.nn.sigmoid(f_pre.astype(f32))
    k = 1.0 - f
    log_f = jnp.log(f)

    def heads(t, dim):
        return t.reshape(B, nb, HG_BLOCK, HG_HEADS, dim).transpose(0, 3, 1, 2, 4)

    qh = heads(q.astype(f32), HG_EXPAND)
    kh = heads(k, HG_EXPAND)
    vh = heads(i_in.astype(f32), HG_V_DIM)
    b = jnp.cumsum(heads(log_f, HG_EXPAND), axis=3)
    b_last = b[:, :, :, -1:, :]
    b_mid = b[:, :, :, HG_MID:HG_MID + 1, :]

    q_r = qh * jnp.exp(b - b_mid)
    k_r = kh * jnp.exp(b_mid - b)
    causal = jnp.tril(jnp.ones((HG_BLOCK, HG_BLOCK), dtype=bool))
    a = jnp.where(causal, jnp.einsum('bhntk,bhnsk->bhnts', q_r, k_r), 0.0)
    o_intra = jnp.einsum('bhnts,bhnsv->bhntv', a, vh)

    q_inter = qh * jnp.exp(b)
    k_state = kh * jnp.exp(b_last - b)
    decay = jnp.exp(b_last[:, :, :, 0, :])

    def step(state, xs):
        qi, ks, vs, dec = xs
        o = jnp.einsum('bhtk,bhkv->bhtv', qi, state)
        state = state * dec[..., None] + jnp.einsum('bhtk,bhtv->bhkv', ks, vs)
        return state, o

    xs = (jnp.moveaxis(q_inter, 2, 0), jnp.moveaxis(k_state, 2, 0),
          jnp.moveaxis(vh, 2, 0), jnp.moveaxis(decay, 2, 0))
    s0 = jnp.zeros((B, HG_HEADS, HG_EXPAND, HG_V_DIM), f32)
    _, o_inter = lax.scan(step, s0, xs)

    o = o_intra + jnp.moveaxis(o_inter, 0, 2)
    o = o.transpose(0, 2, 3, 1, 4).reshape(B, S, HG_HEADS, HG_V_DIM)
    gate = jax.nn.silu(g.astype(f32).reshape(B, S, HG_HEADS, HG_V_DIM))
    o = rmsnorm(o, gnorm_w) * gate
    return o.reshape(B, S, D_MODEL).astype(x.dtype) @ w_out


def diff_attention(x, w_in, lam, subln_w, w_out, layer):
    f32 = jnp.float32
    B, S, _ = x.shape
    H, dh = DA_HEADS, DA_HEAD_DIM
    q, k, v = jnp.split(x @ w_in, 3, axis=-1)
    q = q.reshape(B, S, H, 2, dh).transpose(0, 2, 3, 1, 4)
    k = k.reshape(B, S, H, 2, dh).transpose(0, 2, 3, 1, 4)
    v = v.reshape(B, S, H, 2 * dh).transpose(0, 2, 1, 3)

    lam_init = 0.8 - 0.6 * math.exp(-0.3 * layer)
    lam_f = lam.astype(f32)
    lam_full = (jnp.exp(jnp.sum(lam_f[0] * lam_f[1]))
                - jnp.exp(jnp.sum(lam_f[2] * lam_f[3])) + lam_init)
    slopes = alibi_slopes(H)
    scale = dh ** -0.5
    n_qb = S // DA_Q_BLOCK
    q_blocks = q.reshape(B, H, 2, n_qb, DA_Q_BLOCK, dh).transpose(3, 0, 1, 2, 4, 5)
    key_pos = jnp.arange(S)

    def block(args):
        qb, start = args
        q_pos = start + jnp.arange(DA_Q_BLOCK)
        s = jnp.einsum('bhgqd,bhgkd->bhgqk', qb, k).astype(f32) * scale
        dist = jnp.abs(q_pos[:, None] - key_pos[None, :]).astype(f32)
        allowed = (key_pos[None, :] // CHUNK) <= (q_pos[:, None] // CHUNK)
        s = s - slopes[None, :, None, None, None] * dist
        p = jax.nn.softmax(jnp.where(allowed, s, -jnp.inf), axis=-1)
        w = p[:, :, 0] - lam_full * p[:, :, 1]
        return jnp.einsum('bhqk,bhkv->bhqv', w.astype(v.dtype), v)

    starts = jnp.arange(n_qb, dtype=jnp.int32) * DA_Q_BLOCK
    o = lax.map(block, (q_blocks, starts))
    o = o.transpose(1, 0, 3, 2, 4).reshape(B, S, H, 2 * dh)
    o = rmsnorm(o, subln_w) * (1.0 - lam_init)
    return o.reshape(B, S, D_MODEL) @ w_out


def pool_mixer(x, w_in, w_group, scale, w_out):
    f32 = jnp.float32
    B, S, _ = x.shape
    u = (x @ w_in).astype(f32).reshape(B, S, POOL_GROUPS, POOL_GROUP_DIM)
    csum = jnp.cumsum(u, axis=1)
    count_pos = jnp.arange(S)
    outs = []
    for g_idx, w in enumerate(POOL_WINDOWS):
        cs = csum[:, :, g_idx]
        lag = jnp.pad(cs[:, :S - w], ((0, 0), (w, 0), (0, 0)))
        count = jnp.minimum(count_pos + 1, w).astype(f32)
        mean = (cs - lag) / count[None, :, None]
        outs.append(mean - u[:, :, g_idx])
    p = jnp.stack(outs, axis=2)
    y = jnp.einsum('bsgc,gcd->bsgd', p, w_group.astype(f32))
    y = y * scale.astype(f32).reshape(POOL_GROUPS, POOL_GROUP_DIM)
    return y.reshape(B, S, D_MODEL).astype(x.dtype) @ w_out


def setup_inputs(seed: int = 0) -> dict:
    key = jax.random.key(seed)
    ks = jax.random.split(key, 18)
    f32 = jnp.float32

    def dense(k, shape, fan_in):
        return jax.random.normal(k, shape, f32) * fan_in ** -0.5

    def gain(k, shape, s=0.05):
        return 1.0 + s * jax.random.normal(k, shape, f32)

    G, C = POOL_GROUPS, POOL_GROUP_DIM
    return {
        "x": jax.random.normal(ks[0], (BATCH, SEQ, D_MODEL), f32),
        "norm_gains": gain(ks[1], (DEPTH, N_SUBLAYER_NORMS, D_MODEL)),
        "ffn1_w_in": dense(ks[2], (DEPTH, D_MODEL, 2 * D_FF), D_MODEL),
        "ffn1_w_out": dense(ks[3], (DEPTH, D_FF, D_MODEL), D_FF),
        "ffn2_w_in": dense(ks[4], (DEPTH, D_MODEL, 2 * D_FF), D_MODEL),
        "ffn2_w_out": dense(ks[5], (DEPTH, D_FF, D_MODEL), D_FF),
        "hgrn_w_in": dense(ks[6], (N_A, D_MODEL, 2 * HG_F_DIM + 2 * D_MODEL), D_MODEL),
        "hgrn_gnorm": gain(ks[7], (N_A, HG_V_DIM)),
        "hgrn_w_out": dense(ks[8], (N_A, D_MODEL, D_MODEL), D_MODEL),
        "hgrn_lb_raw": 0.5 * jax.random.normal(ks[9], (DEPTH, HG_F_DIM), f32),
        "diff_w_in": dense(ks[10], (N_B, D_MODEL, 3 * D_MODEL), D_MODEL),
        "diff_lambda": 0.1 * jax.random.normal(ks[11], (N_B, 4, DA_HEAD_DIM), f32),
        "diff_subln": gain(ks[12], (N_B, 2 * DA_HEAD_DIM)),
        "diff_w_out": dense(ks[13], (N_B, D_MODEL, D_MODEL), D_MODEL),
        "pool_w_in": dense(ks[14], (N_C, D_MODEL, D_MODEL), D_MODEL),
        "pool_w_group": dense(ks[15], (N_C, G, C, C), C),
        "pool_scale": gain(ks[16], (N_C, D_MODEL), 0.1),
        "pool_w_out": dense(ks[17], (N_C, D_MODEL, D_MODEL), D_MODEL),
    }


def reference(x, norm_gains, ffn1_w_in, ffn1_w_out, ffn2_w_in, ffn2_w_out,
              hgrn_w_in, hgrn_gnorm, hgrn_w_out, hgrn_lb_raw,
              diff_w_in, diff_lambda, diff_subln, diff_w_out,
              pool_w_in, pool_w_group, pool_scale, pool_w_out):
    h = x
    for i in range(DEPTH):
        kind, j = i % N_MIXERS, i // N_MIXERS
        gn = norm_gains[i]
        h = h + 0.5 * rmsnorm(swiglu(rmsnorm(h, gn[0]), ffn1_w_in[i], ffn1_w_out[i]), gn[1])
        hn = rmsnorm(h, gn[2])
        if kind == 0:
            lb = hgrn_lower_bound(hgrn_lb_raw, i)
            m = hgrn2_mixer(hn, hgrn_w_in[j], hgrn_gnorm[j], hgrn_w_out[j], lb)
        elif kind == 1:
            m = diff_attention(hn, diff_w_in[j], diff_lambda[j], diff_subln[j], diff_w_out[j], i)
        else:
            m = pool_mixer(hn, pool_w_in[j], pool_w_group[j], pool_scale[j], pool_w_out[j])
        h = h + rmsnorm(m, gn[3])
        h = h + 0.5 * rmsnorm(swiglu(rmsnorm(h, gn[4]), ffn2_w_in[i], ffn2_w_out[i]), gn[5])
    return h
```

```python
import math
import numpy as np
import concourse.bass as bass
import concourse.mybir as mybir
from concourse.bass_utils import run_bass_kernel_spmd

F32 = mybir.dt.float32
BF16 = mybir.dt.bfloat16
AF = mybir.ActivationFunctionType
ALU = mybir.AluOpType
AX = mybir.AxisListType

D = 1024
NC8 = 8
DFF = 2816
NJ = 22
DEPTH = 4
EPS = 1e-6
ENGS = ['pe', 'act', 'dve', 'pool', 'sp']
SAME_SYNC = {'pe': False, 'act': True, 'dve': True, 'pool': True, 'sp': True}
EPOCH = 30000
NDSEM = 16


class Op:
    __slots__ = ('eng', 'fn', 'deps', 'dma', 'dsem', 'dval', 'signal', 'count', 'idx')


class Buf:
    __slots__ = ('name', 'w', 'r', 'lo', 'hi')

    def __init__(self, name=''):
        self.name = name
        self.w = {}
        self.r = {}


def _key(o):
    return ('d', o.dsem) if o.dma else o.eng


def _ord(o):
    return o.dval if o.dma else o.idx


class Prog:
    def __init__(self, nc):
        self.nc = nc
        self.ops = {e: [] for e in ENGS}
        self.dsem_val = [0] * NDSEM
        self.dsem_last = [None] * NDSEM
        self.next_dsem = {'sp': 0, 'pool': 0, 'act': 0}

    def op(self, eng, fn, reads=(), writes=(), dma=False):
        o = Op()
        o.eng = eng; o.fn = fn; o.dma = dma; o.signal = False; o.count = None
        o.idx = len(self.ops[eng]); o.dsem = None; o.dval = None
        deps = {}

        def add(d):
            k = _key(d)
            c = deps.get(k)
            if c is None or _ord(d) > _ord(c):
                deps[k] = d
        for b in reads:
            for d in b.w.values():
                add(d)
        for b in writes:
            for d in b.w.values():
                add(d)
            for d in b.r.values():
                add(d)
        if dma:
            half = NDSEM // 2
            base = 0 if eng == 'sp' else half
            s = base + self.next_dsem[eng]
            self.next_dsem[eng] = (self.next_dsem[eng] + 1) % half
            if self.dsem_last[s] is not None:
                add(self.dsem_last[s])
            o.dsem = s
            self.dsem_val[s] += 16
            o.dval = self.dsem_val[s]
            self.dsem_last[s] = o
        o.deps = [d for d in deps.values() if d.dma or d.eng != eng or SAME_SYNC[eng]]
        for d in o.deps:
            if not d.dma:
                d.signal = True
        k = _key(o)
        for b in reads:
            b.r[k] = o
        for b in writes:
            b.w = {k: o}
            b.r = {}
        self.ops[eng].append(o)
        return o

    def emit(self):
        nc = self.nc
        nsig = {}
        for e in ENGS:
            c = 0
            for o in self.ops[e]:
                if o.signal:
                    c += 1
                    o.count = c
            nsig[e] = c
        esems = {e: [nc.alloc_semaphore(f"s_{e}_{i}") for i in range(max(1, (nsig[e] + EPOCH - 1) // EPOCH))]
                 for e in ENGS}
        dsems = [nc.alloc_semaphore(f"s_dma_{i}") for i in range(NDSEM)]

        def semval(d):
            if d.dma:
                return ('d', d.dsem), dsems[d.dsem], d.dval
            ep = (d.count - 1) // EPOCH
            return (d.eng, ep), esems[d.eng][ep], d.count - ep * EPOCH

        ops = self.ops
        dsem_val = self.dsem_val

        def run(eng, e):
            waited = {}
            for o in ops[eng]:
                for d in o.deps:
                    k, s, v = semval(d)
                    if waited.get(k, 0) >= v:
                        continue
                    waited[k] = v
                    e.wait_ge(s, v)
                ins = o.fn(e)
                if o.dma:
                    ins.then_inc(dsems[o.dsem], 16)
                elif o.signal:
                    _, s, _ = semval(o)
                    ins.then_inc(s, 1)
            if eng == 'sp':
                for i in range(NDSEM):
                    if dsem_val[i] > 0:
                        e.wait_ge(dsems[i], dsem_val[i])

        with nc.Block() as block:
            @block.tensor
            def _(e):
                run('pe', e)

            @block.scalar
            def _(e):
                run('act', e)

            @block.vector
            def _(e):
                run('dve', e)

            @block.gpsimd
            def _(e):
                run('pool', e)

            @block.sync
            def _(e):
                run('sp', e)


class Region:
    def __init__(self):
        self.bufs = []

    def alloc(self, lo, n, name=''):
        b = Buf(name)
        b.lo = lo; b.hi = lo + n
        for o in self.bufs:
            if o.lo < b.hi and b.lo < o.hi:
                for k, d in o.w.items():
                    c = b.w.get(k)
                    if c is None or _ord(d) > _ord(c):
                        b.w[k] = d
                for k, d in o.r.items():
                    c = b.r.get(k)
                    if c is None or _ord(d) > _ord(c):
                        b.r[k] = d
        self.bufs.append(b)
        return b


def pack_cols(W, col_groups):
    K = W.shape[0]
    KC = K // 128
    Wr = W.reshape(KC, 128, W.shape[1])
    outs = []
    for cols in col_groups:
        blk = Wr[:, :, cols]
        outs.append(np.ascontiguousarray(blk.transpose(1, 0, 2)).reshape(128, -1))
    return outs


class WStream:
    def __init__(self):
        self.parts = []
        self.off = 0

    def add(self, arr):
        o = self.off
        self.parts.append(arr)
        self.off += arr.shape[1]
        return (o, arr.shape[1])

    def array(self):
        return np.ascontiguousarray(np.concatenate(self.parts, axis=1).astype(np.float32))


def ffn_groups():
    gs = []
    for s in range(11):
        c0 = s * 256
        gs.append(np.concatenate([np.arange(c0, c0 + 256), DFF + np.arange(c0, c0 + 256)]))
    return gs


def out_groups(n_out=D):
    return [np.arange(m * 128, (m + 1) * 128) for m in range(n_out // 128)]


def plan_weights(inputs):
    ws = WStream()
    plan = []
    for i in range(DEPTH):
        kind, j = i % 3, i // 3
        L = {}
        for nm, win, wout in (('f1', 'ffn1_w_in', 'ffn1_w_out'), ('f2', 'ffn2_w_in', 'ffn2_w_out')):
            L[nm + '_in'] = [ws.add(a) for a in pack_cols(inputs[win][i], ffn_groups())]
            L[nm + '_out'] = [ws.add(a) for a in pack_cols(inputs[wout][i], out_groups())]
        if kind == 0:
            W = inputs['hgrn_w_in'][j]
            L['m_in'] = [ws.add(a) for a in pack_cols(W, [np.arange(c * 512, (c + 1) * 512) for c in range(8)])]
            L['m_out'] = [ws.add(a) for a in pack_cols(inputs['hgrn_w_out'][j], out_groups())]
        elif kind == 1:
            W = inputs['diff_w_in'][j]
            L['m_in'] = [ws.add(a) for a in pack_cols(W, [np.arange(c * 512, (c + 1) * 512) for c in range(6)])]
            L['m_out'] = [ws.add(a) for a in pack_cols(inputs['diff_w_out'][j], out_groups())]
        else:
            L['m_in'] = [ws.add(a) for a in pack_cols(inputs['pool_w_in'][j], [np.arange(c * 512, (c + 1) * 512) for c in range(2)])]
            wg = inputs['pool_w_group'][j]
            L['m_grp'] = [ws.add(pack_cols(wg[g], [np.arange(256)])[0]) for g in range(4)]
            L['m_out'] = [ws.add(a) for a in pack_cols(inputs['pool_w_out'][j], out_groups())]
        plan.append(L)
    return ws, plan


C_GAIN = 0
C_GNORM = 192
C_SUBLN = 194
C_PSCALE = 195
C_LBRAW = 203
C_LAM = 235
C_IDENT = 491
C_HMASK = 619
C_PINV = 747
C_BLKM = 811
NCST = 819


def pack_consts(inp):
    c = np.zeros((128, NCST), np.float32)
    c[:, C_GAIN:C_GAIN + 192] = inp['norm_gains'].reshape(4, 6, 8, 128).transpose(3, 0, 1, 2).reshape(128, 192)
    c[:, C_GNORM:C_GNORM + 2] = inp['hgrn_gnorm'].T
    c[:, C_SUBLN:C_SUBLN + 1] = inp['diff_subln'].T
    c[:, C_PSCALE:C_PSCALE + 8] = inp['pool_scale'][0].reshape(8, 128).T
    c[:, C_LBRAW:C_LBRAW + 32] = inp['hgrn_lb_raw'].reshape(4, 8, 128).transpose(2, 1, 0).reshape(128, 32)
    c[:, C_LAM:C_LAM + 256] = np.broadcast_to(inp['diff_lambda'][0].reshape(1, 256), (128, 256))
    c[:, C_IDENT:C_IDENT + 128] = np.eye(128, dtype=np.float32)
    s = np.arange(128)[:, None]; t = np.arange(128)[None, :]
    c[:, C_HMASK:C_HMASK + 128] = ((s // 16 == t // 16) & (s <= t)).astype(np.float32)
    pinv = np.zeros((4, 16), np.float32)
    for g, w in enumerate((2, 4, 8, 16)):
        pinv[g] = 1.0 / np.minimum(np.arange(16) + 1, w)
    c[:, C_PINV:C_PINV + 64] = np.broadcast_to(pinv.reshape(1, 64), (128, 64))
    c[:, C_BLKM:C_BLKM + 8] = (np.arange(128)[:, None] // 16 == np.arange(8)[None, :]).astype(np.float32)
    return c


class Ctx:
    pass


def build(S, T, plan, wtot, sublayers, first_src_is_x=True):
    nc = bass.Bass("TRN2", target_bir_lowering=False)
    P = Prog(nc)
    NT = S // T
    xT = nc.dram_tensor("xT", [D, S], F32, kind="ExternalInput").ap()
    wst = nc.dram_tensor("wst", [128, wtot], F32, kind="ExternalInput").ap()
    cstd = nc.dram_tensor("cst", [128, NCST], F32, kind="ExternalInput").ap()
    oT = nc.dram_tensor("oT", [D, S], F32, kind="ExternalOutput").ap()
    hscr = nc.dram_tensor("hscr", [D, S], F32).ap()
    NR = T // 128
    atab = nc.dram_tensor("atab", [128, (NR + 1) * T], F32, kind="ExternalInput").ap()
    ktd = nc.dram_tensor("ktd", [8, 128, S], BF16).ap()
    vd = nc.dram_tensor("vd", [8, 128, S // 128, 128], BF16).ap()

    A_CST = 0
    A_MISC = 3328
    A_H = A_MISC + 1280
    A_XN = A_H + 32 * T
    A_FS = A_XN + 16 * T
    A_BS = A_FS + 48 * T
    A_W = A_BS + 44 * T
    WBYTES = 135168
    A_END = A_W + WBYTES
    arena = nc.alloc_sbuf_tensor("arena", [128, A_END // 2], BF16)
    R = Region()

    def vb(off, n):
        return arena[:, off // 2: off // 2 + n]

    def vf(off, n):
        return arena[:, off // 2: off // 2 + 2 * n].bitcast(F32)

    c = Ctx()
    c.nc = nc; c.P = P; c.R = R; c.T = T; c.S = S; c.NT = NT
    c.vb = vb; c.vf = vf; c.wst = wst
    c.atab = atab; c.ktd = ktd; c.vd = vd
    c.A_MISC = A_MISC; c.A_W = A_W; c.A_FS = A_FS; c.A_BS = A_BS; c.A_XN = A_XN; c.A_H = A_H; c.WBYTES = WBYTES
    cst = vf(A_CST, NCST)
    c.cst = cst
    cstb = R.alloc(A_CST, NCST * 4, 'cst')
    c.cstb = cstb
    c.P = P
    DMA(c, 'sp', cst, cstd[:, :], [], [cstb])
    ones_d = vb(A_MISC, 128); ones_h = vb(A_MISC + 256, 128); identb = vb(A_MISC + 512, 128)
    miscb = R.alloc(A_MISC, 1280, "misc")
    c.ones_d = ones_d; c.ones_h = ones_h; c.identb = identb; c.miscb = miscb
    c.identf = cst[:, C_IDENT:C_IDENT + 128]
    c.epsc = vf(A_MISC + 768, 1)
    MEMSET(c, c.epsc, EPS, [miscb])
    MEMSET(c, ones_d, 1.0 / D, [miscb])
    MEMSET(c, ones_h, 1.0 / 128, [miscb])
    CP(c, identb, cst[:, C_IDENT:C_IDENT + 128], [cstb], [miscb])

    c.ps = [nc.alloc_psum_tensor(f"ps{i}", [128, 512], F32) for i in range(8)]
    c.PR = Region()
    c.psi = 0
    c.ps_lim = 8

    def reset_ps():
        c.psb = [c.PR.alloc(i * 2048, 2048, f"ps{i}") for i in range(8)]
        c.ps_lim = 8
        c.psi = 0
        c.ps_base = 0
    c.reset_ps = reset_ps
    reset_ps()

    c.ps_base = 0

    def next_ps():
        i = c.psi % c.ps_lim
        c.psi = (i + 1) % c.ps_lim
        return c.ps[c.ps_base + i], c.psb[c.ps_base + i]
    c.next_ps = next_ps

    c.H = vf(A_H, 8 * T).rearrange("p (c t) -> p c t", t=T)
    c.Hflat = vf(A_H, 8 * T)
    c.Hb = R.alloc(A_H, 32 * T, 'H')
    c.XN = vb(A_XN, 8 * T).rearrange("p (c t) -> p c t", t=T)
    c.XNflat = vb(A_XN, 8 * T)
    c.XNb = R.alloc(A_XN, 16 * T, 'XN')

    dt = {}

    def dtile(name, i):
        k = (name, i)
        if k not in dt:
            dt[k] = Buf(f"{name}{i}")
        return dt[k]

    tens = {'x': xT, 'h': hscr, 'o': oT}
    nsl = len(sublayers)
    c.state = {}
    for si, (layer, kind) in enumerate(sublayers):
        src = 'x' if si == 0 else 'h'
        dst = 'o' if si == nsl - 1 else 'h'
        L = plan[layer]
        mk = layer % 3
        c.reset_ps()
        if kind in ('f1', 'f2'):
            sub = FFNSub(c, L[kind + '_in'], L[kind + '_out'], layer, 0 if kind == 'f1' else 4)
        elif mk == 2:
            sub = PoolSub(c, L, layer)
        elif mk == 0:
            sub = HgrnSub(c, L, layer)
        else:
            sub = AttnSub(c, L, layer)
        for ti in range(NT):
            t0 = ti * T
            sa = tens[src].rearrange("(c p) s -> p c s", p=128)[:, :, t0:t0 + T]
            DMA(c, 'sp', c.H, sa, [dtile(src, ti)], [c.Hb])
            sub.tile(ti)
            da = tens[dst].rearrange("(c p) s -> p c s", p=128)[:, :, t0:t0 + T]
            DMA(c, 'sp', da, c.H, [c.Hb], [dtile(dst, ti)])
    P.emit()
    return nc


def MM(c, out, lhsT, rhs, start, stop, reads, writes):
    return c.P.op('pe', lambda e: e.matmul(out, lhsT, rhs, start=start, stop=stop), reads, writes)


def TR(c, out, in_, ident, reads, writes):
    return c.P.op('pe', lambda e: e.transpose(out, in_, ident), reads, writes)


def ACT(c, out, in_, func, reads, writes, bias=0.0, scale=1.0):
    return c.P.op('act', lambda e: e.activation(out=out, in_=in_, func=func, bias=bias, scale=scale), reads, writes)


def TT(c, out, in0, in1, op, reads, writes, eng='dve'):
    return c.P.op(eng, lambda e: e.tensor_tensor(out=out, in0=in0, in1=in1, op=op), reads, writes)


def STT(c, out, in0, scalar, in1, op0, op1, reads, writes, eng='dve'):
    return c.P.op(eng, lambda e: e.scalar_tensor_tensor(out=out, in0=in0, scalar=scalar, in1=in1, op0=op0, op1=op1), reads, writes)


def TS(c, out, in0, s1, s2, op0, op1, reads, writes, eng='dve'):
    if s2 is None:
        return c.P.op(eng, lambda e: e.tensor_scalar(out=out, in0=in0, scalar1=s1, scalar2=None, op0=op0), reads, writes)
    return c.P.op(eng, lambda e: e.tensor_scalar(out=out, in0=in0, scalar1=s1, scalar2=s2, op0=op0, op1=op1), reads, writes)


def CP(c, out, in_, reads, writes, eng='dve'):
    return c.P.op(eng, lambda e: e.tensor_copy(out=out, in_=in_), reads, writes)


def RECIP(c, out, in_, reads, writes):
    return c.P.op('dve', lambda e: e.reciprocal(out=out, in_=in_), reads, writes)


def MEMSET(c, out, val, writes, eng='pool'):
    return c.P.op(eng, lambda e: e.memset(out, val), (), writes)


def DMA(c, eng, out, in_, reads, writes):
    return c.P.op(eng, lambda e: e.dma_start(out=out, in_=in_), reads, writes, dma=True)


def load_slots(c, slots, base, stride_bytes, shape_fn, name):
    out = []
    for i, (off, n) in enumerate(slots):
        lo = base + i * stride_bytes
        assert n * 2 <= stride_bytes
        assert lo + n * 2 <= c.A_W + c.WBYTES, (name, i)
        b = c.R.alloc(lo, n * 2, f"{name}{i}")
        v = c.vb(lo, n)
        DMA(c, 'pool', v, c.wst[:, off:off + n], [], [b])
        out.append((shape_fn(v), b))
    return out


def rstd_from_ps(c, ps, psb, rstd_view, rstd_buf, n=None):
    n = c.T if n is None else n
    ACT(c, rstd_view, ps[:, 0:n], AF.Sqrt, [psb, c.miscb], [rstd_buf], bias=c.epsc)
    RECIP(c, rstd_view, rstd_view, [rstd_buf], [rstd_buf])


def rms_stats(c, sq_view, sq_bufs, nchunks, ones, rstd_view, rstd_buf):
    T = c.T
    ps, psb = c.next_ps()
    for k in range(nchunks):
        MM(c, ps[:, 0:T], ones, sq_view[:, k, :], k == 0, k == nchunks - 1, list(sq_bufs) + [c.miscb], [psb])
    rstd_from_ps(c, ps, psb, rstd_view, rstd_buf)


def prenorm(c, gcol, sq_view, sq_bufs, rstd_view, rstd_buf):
    ACT(c, sq_view, c.H, AF.Square, [c.Hb], list(sq_bufs))
    rms_stats(c, sq_view, sq_bufs, 8, c.ones_d, rstd_view, rstd_buf)
    for k in range(8):
        g = c.cst[:, gcol + k: gcol + k + 1]
        STT(c, c.XN[:, k, :], c.H[:, k, :], g, rstd_view, ALU.mult, ALU.mult, [c.Hb, rstd_buf, c.cstb], [c.XNb])


def postnorm_residual(c, gcol, Y, Yflat, Yb, sq_view, sq_bufs, rstd_view, rstd_buf, factor):
    rms_stats(c, sq_view, sq_bufs, 8, c.ones_d, rstd_view, rstd_buf)
    for k in range(8):
        g = c.cst[:, gcol + k: gcol + k + 1]
        STT(c, Y[:, k, :], Y[:, k, :], g, rstd_view, ALU.mult, ALU.mult, [Yb, rstd_buf, c.cstb], [Yb])
    STT(c, c.Hflat, Yflat, float(factor), c.Hflat, ALU.mult, ALU.add, [Yb, c.Hb], [c.Hb])


class FFNSub:
    def __init__(self, c, slots_in, slots_out, layer, nbase):
        self.c = c
        T = c.T
        self.gpre = C_GAIN + (layer * 6 + nbase) * 8
        self.gpost = C_GAIN + (layer * 6 + nbase + 1) * 8
        self.win = load_slots(c, slots_in, c.A_W, 8192, lambda v: v.rearrange("p (k n) -> p k n", n=512), 'wi')
        self.wout = load_slots(c, slots_out, c.A_W + 11 * 8192, 5632, lambda v: v.rearrange("p (k n) -> p k n", n=128), 'wo')
        R = c.R
        self.Y = c.vf(c.A_FS, 8 * T).rearrange("p (c t) -> p c t", t=T)
        self.Yflat = c.vf(c.A_FS, 8 * T)
        self.Yb = R.alloc(c.A_FS, 32 * T, 'Y')
        self.rstd = c.vf(c.A_FS + 32 * T, T)
        self.rstdb = R.alloc(c.A_FS + 32 * T, 4 * T, 'rstd')
        self.sg = [c.vf(c.A_FS + 36 * T + 4 * T * i, T) for i in range(2)]
        self.sgb = [R.alloc(c.A_FS + 36 * T + 4 * T * i, 4 * T, f'sg{i}') for i in range(2)]
        self.hid = c.vb(c.A_BS, NJ * T).rearrange("p (c t) -> p c t", t=T)
        self.hidb = [R.alloc(c.A_BS + 2 * T * j, 2 * T, f'hid{j}') for j in range(NJ)]

    def tile(self, ti):
        c = self.c; T = c.T
        prenorm(c, self.gpre, self.hid[:, 0:8, :], self.hidb[0:8], self.rstd, self.rstdb)
        for s in range(11):
            wv, wb = self.win[s]
            for jj in range(2):
                j = 2 * s + jj
                psg, psgb = c.next_ps()
                for k in range(8):
                    MM(c, psg[:, 0:T], wv[:, k, jj * 128:(jj + 1) * 128], c.XN[:, k, :], k == 0, k == 7, [wb, c.XNb], [psgb])
                psu, psub = c.next_ps()
                for k in range(8):
                    MM(c, psu[:, 0:T], wv[:, k, 256 + jj * 128:256 + (jj + 1) * 128], c.XN[:, k, :], k == 0, k == 7, [wb, c.XNb], [psub])
                sg = self.sg[j % 2]; sgb = self.sgb[j % 2]
                ACT(c, sg, psg[:, 0:T], AF.Silu, [psgb], [sgb])
                TT(c, self.hid[:, j, :], sg, psu[:, 0:T], ALU.mult, [sgb, psub], [self.hidb[j]])
        for m in range(8):
            wv, wb = self.wout[m]
            ps, psb = c.next_ps()
            for j in range(NJ):
                MM(c, ps[:, 0:T], wv[:, j, :], self.hid[:, j, :], j == 0, j == NJ - 1, [wb, self.hidb[j]], [psb])
            ACT(c, self.Y[:, m, :], ps[:, 0:T], AF.Copy, [psb], [self.Yb])
            ACT(c, c.XN[:, m, :], ps[:, 0:T], AF.Square, [psb], [c.XNb])
        postnorm_residual(c, self.gpost, self.Y, self.Yflat, self.Yb, c.XN, [c.XNb], self.rstd, self.rstdb, 0.5)


def out_proj_residual(c, wout, IN, INbufs, gpost, Y, Yflat, Yb, rstd, rstdb, factor=1.0):
    T = c.T
    for m in range(8):
        wv, wb = wout[m]
        ps, psb = c.next_ps()
        for k in range(8):
            MM(c, ps[:, 0:T], wv[:, k, :], IN[:, k, :], k == 0, k == 7, [wb] + list(INbufs), [psb])
        ACT(c, Y[:, m, :], ps[:, 0:T], AF.Copy, [psb], [Yb])
        ACT(c, c.XN[:, m, :], ps[:, 0:T], AF.Square, [psb], [c.XNb])
    postnorm_residual(c, gpost, Y, Yflat, Yb, c.XN, [c.XNb], rstd, rstdb, factor)


class MixBase:
    def common(self, c, layer):
        T = c.T; R = c.R
        self.c = c
        self.gpre = C_GAIN + (layer * 6 + 2) * 8
        self.gpost = C_GAIN + (layer * 6 + 3) * 8
        self.Y = c.vf(c.A_FS, 8 * T).rearrange("p (c t) -> p c t", t=T)
        self.Yflat = c.vf(c.A_FS, 8 * T)
        self.Yb = R.alloc(c.A_FS, 32 * T, 'Y')
        self.rstd = c.vf(c.A_FS + 32 * T, T)
        self.rstdb = R.alloc(c.A_FS + 32 * T, 4 * T, 'rstd')
        self.YV = c.vb(c.A_BS, 8 * T).rearrange("p (c t) -> p c t", t=T)
        self.YVb = [R.alloc(c.A_BS + 2 * T * k, 2 * T, f'YV{k}') for k in range(8)]
        self.SQ = self.YV
        self.SQb = self.YVb


class PoolSub(MixBase):
    def __init__(self, c, L, layer):
        self.common(c, layer)
        T = c.T; R = c.R
        W0 = c.A_W
        self.win = load_slots(c, L['m_in'], W0, 8192, lambda v: v.rearrange("p (k n) -> p k n", n=512), 'pwi')
        self.wgrp = load_slots(c, L['m_grp'], W0 + 16384, 1024, lambda v: v.rearrange("p (k n) -> p k n", n=256), 'pwg')
        self.wout = load_slots(c, L['m_out'], W0 + 20480, 2048, lambda v: v.rearrange("p (k n) -> p k n", n=128), 'pwo')
        E = 16 + T
        self.E = E
        base = W0 + 40960
        self.U = []; self.Ub = []; self.TA = []; self.TAb = []; self.TB = []; self.TBb = []
        for k in range(8):
            for lst, lstb, o in ((self.U, self.Ub, 0), (self.TA, self.TAb, 1), (self.TB, self.TBb, 2)):
                off = base + (o * 8 + k) * 4 * E
                lst.append(c.vf(off, E))
                lstb.append(R.alloc(off, 4 * E, f'pool{o}_{k}'))
        self.PB = c.vb(c.A_BS + 16 * T, 8 * T).rearrange("p (c t) -> p c t", t=T)
        self.PBb = [R.alloc(c.A_BS + 16 * T + 2 * T * k, 2 * T, f'PB{k}') for k in range(8)]
        for k in range(8):
            MEMSET(c, self.U[k][:, 0:16], 0.0, [self.Ub[k]])

    def tile(self, ti):
        c = self.c; T = c.T; E = self.E
        prenorm(c, self.gpre, self.SQ, self.SQb, self.rstd, self.rstdb)
        for m in range(8):
            U = self.U[m]; Ub = self.Ub[m]
            import os
            if ti > 0 and not os.environ.get('NOHALO'):
                CP(c, U[:, 0:16], U[:, T:T + 16], [Ub], [Ub])
            wv, wb = self.win[m // 4]
            ps, psb = c.next_ps()
            for k in range(8):
                MM(c, ps[:, 0:T], wv[:, k, (m % 4) * 128:(m % 4 + 1) * 128], c.XN[:, k, :], k == 0, k == 7, [wb, c.XNb], [psb])
            ACT(c, U[:, 16:E], ps[:, 0:T], AF.Copy, [psb], [Ub])
            g = m // 2
            TA = self.TA[m]; TAb = self.TAb[m]; TB = self.TB[m]; TBb = self.TBb[m]
            TT(c, TA[:, 1:E], U[:, 1:E], U[:, 0:E - 1], ALU.add, [Ub], [TAb])
            Wv, Wb = TA, TAb
            if g >= 1:
                TT(c, TB[:, 3:E], TA[:, 3:E], TA[:, 1:E - 2], ALU.add, [TAb], [TBb])
                Wv, Wb = TB, TBb
            if g >= 2:
                TT(c, TA[:, 7:E], TB[:, 7:E], TB[:, 3:E - 4], ALU.add, [TBb], [TAb])
                Wv, Wb = TA, TAb
            if g >= 3:
                TT(c, TB[:, 15:E], TA[:, 15:E], TA[:, 7:E - 8], ALU.add, [TAb], [TBb])
                Wv, Wb = TB, TBb
            w = 2 ** (g + 1)
            lo = 0
            if ti == 0:
                pinv = c.cst[:, C_PINV + g * 16: C_PINV + (g + 1) * 16]
                TT(c, Wv[:, 16:32], Wv[:, 16:32], pinv, ALU.mult, [Wb, c.cstb], [Wb])
                TT(c, self.PB[:, m, 0:16], Wv[:, 16:32], U[:, 16:32], ALU.subtract, [Wb, Ub], [self.PBb[m]])
                lo = 16
            STT(c, self.PB[:, m, lo:T], Wv[:, 16 + lo:E], 1.0 / w, U[:, 16 + lo:E], ALU.mult, ALU.subtract, [Wb, Ub], [self.PBb[m]])
        for g in range(4):
            wv, wb = self.wgrp[g]
            for oc in range(2):
                mo = 2 * g + oc
                ps, psb = c.next_ps()
                for k2 in range(2):
                    MM(c, ps[:, 0:T], wv[:, k2, oc * 128:(oc + 1) * 128], self.PB[:, 2 * g + k2, :], k2 == 0, k2 == 1,
                       [wb, self.PBb[2 * g + k2]], [psb])
                TS(c, self.YV[:, mo, :], ps[:, 0:T], c.cst[:, C_PSCALE + mo:C_PSCALE + mo + 1], None, ALU.mult, None, [psb, c.cstb], [self.YVb[mo]])
        out_proj_residual(c, self.wout, self.YV, self.YVb, self.gpost, self.Y, self.Yflat, self.Yb, self.rstd, self.rstdb)


class HgrnSub(MixBase):
    def __init__(self, c, L, layer):
        self.common(c, layer)
        T = c.T; R = c.R
        self.layer = layer
        self.j = layer // 3
        W0 = c.A_W
        self.win = load_slots(c, L['m_in'], W0, 8192, lambda v: v.rearrange("p (k n) -> p k n", n=512), 'hwi')
        self.wout = load_slots(c, L['m_out'], W0 + 65536, 2048, lambda v: v.rearrange("p (k n) -> p k n", n=128), 'hwo')
        o = W0 + 81920
        self.St = []; self.Sb = []
        for hd in range(8):
            self.St.append(c.vf(o, 128)); self.Sb.append(R.alloc(o, 512, f'S{hd}')); o += 512
            MEMSET(c, self.St[hd], 0.0, [self.Sb[hd]])
        self.lb = c.vf(o, 8); self.oml = c.vf(o + 32, 8); self.lbE = c.vf(o + 64, 32); self.lbden = c.vf(o + 192, 8)
        self.lbb = R.alloc(o, 256, 'lb'); o += 256
        self.smask = c.vf(o, T); self.smb = R.alloc(o, 4 * T, 'smask'); o += 4 * T
        MEMSET(c, self.smask, 1.0, [self.smb])
        MEMSET(c, self.smask.rearrange("p (b l) -> p b l", l=16)[:, :, 0:1], 0.0, [self.smb])
        raw = c.cst[:, C_LBRAW:C_LBRAW + 32]
        if layer == 0:
            MEMSET(c, self.lb, 0.0, [self.lbb])
        else:
            ACT(c, self.lbE, raw, AF.Exp, [c.cstb], [self.lbb])
            E3 = self.lbE.rearrange("p (c d) -> p c d", d=4)
            c.P.op('dve', lambda e: e.tensor_reduce(out=self.lbden, in_=E3, axis=AX.X, op=ALU.add), [self.lbb], [self.lbb])
            c.P.op('dve', lambda e: e.tensor_reduce(out=self.lb, in_=E3[:, :, 1:layer + 1], axis=AX.X, op=ALU.add), [self.lbb], [self.lbb])
            RECIP(c, self.lbden, self.lbden, [self.lbb], [self.lbb])
            TT(c, self.lb, self.lb, self.lbden, ALU.mult, [self.lbb], [self.lbb])
        TS(c, self.oml, self.lb, -1.0, 1.0, ALU.mult, ALU.add, [self.lbb], [self.lbb])
        G = T // 128
        self.G = G
        self.Vt = c.vb(o, G * 1024).rearrange("p (g n) -> p g n", n=1024)
        self.Vtb = [R.alloc(o + g * 2048, 2048, f'Vt{g}') for g in range(G)]
        o += G * 2048
        free = [[o, c.A_W + c.WBYTES], [c.A_BS + 16 * T, c.A_BS + 44 * T], [c.A_FS + 36 * T, c.A_FS + 48 * T]]

        def take(nbytes):
            for fr in free:
                if fr[1] - fr[0] >= nbytes:
                    a = fr[0]
                    fr[0] += nbytes
                    return a
            raise AssertionError(("hgrn sbuf overflow", nbytes, free))
        self.sets = []
        for par in range(2):
            st = {}
            for nm in ('Q', 'F', 'K', 'B', 'D', 'E', 'QI', 'KS', 'O', 'Gt'):
                a = take(4 * T)
                st[nm] = c.vf(a, T); st[nm + 'b'] = R.alloc(a, 4 * T, f'{nm}{par}')
            for nm in ('QR', 'KR', 'SQh'):
                a = take(2 * T)
                st[nm] = c.vb(a, T); st[nm + 'b'] = R.alloc(a, 2 * T, f'{nm}{par}')
            a = take(max(T // 4, 32))
            st['DEC'] = c.vf(a, T // 16); st['DECb'] = R.alloc(a, max(T // 4, 32), f'DEC{par}')
            a = take(256)
            st['AT'] = c.vb(a, 128); st['ATb'] = R.alloc(a, 256, f'AT{par}')
            a = take(256)
            st['KSt'] = c.vb(a, 128); st['KStb'] = R.alloc(a, 256, f'KSt{par}')
            a = take(2048)
            st['KSm'] = c.vb(a, 1024).rearrange("p (j n) -> p j n", n=128)
            st['KSmb'] = [R.alloc(a + j * 256, 256, f'KSm{par}_{j}') for j in range(8)]
            self.sets.append(st)
        self.gi = 0
        c.ps_lim = 6

    def nq(self):
        ps, psb = self.c.next_ps()
        return ps[:, 0:128], psb

    def tile(self, ti):
        c = self.c; T = c.T; G = self.G
        c.ps_lim = 6
        prenorm(c, self.gpre, self.SQ, self.SQb, self.rstd, self.rstdb)
        for g in range(G):
            for half in range(2):
                wv, wb = self.win[4 + half]
                ps, psb = c.next_ps()
                for k in range(8):
                    MM(c, ps[:, 0:512], c.XN[:, k, g * 128:(g + 1) * 128], wv[:, k, :], k == 0, k == 7, [wb, c.XNb], [psb])
                ACT(c, self.Vt[:, g, half * 512:(half + 1) * 512], ps[:, 0:512], AF.Copy, [psb], [self.Vtb[g]])
        gn = c.cst[:, C_GNORM + self.j:C_GNORM + self.j + 1]
        for hd in range(8):
            st = self.sets[hd % 2]
            hs = (hd % 4) * 128
            Sv = self.St[hd]; Sb = self.Sb[hd]

            def proj(slot, func, out, outb):
                wv, wb = self.win[slot]
                ps, psb = c.next_ps()
                for k in range(8):
                    MM(c, ps[:, 0:T], wv[:, k, hs:hs + 128], c.XN[:, k, :], k == 0, k == 7, [wb, c.XNb], [psb])
                ACT(c, out, ps[:, 0:T], func, [psb], [outb])
            proj(hd // 4, AF.Copy, st['Q'], st['Qb'])
            proj(2 + hd // 4, AF.Sigmoid, st['F'], st['Fb'])
            proj(6 + hd // 4, AF.Silu, st['Gt'], st['Gtb'])
            TS(c, st['F'], st['F'], self.oml[:, hd:hd + 1], self.lb[:, hd:hd + 1], ALU.mult, ALU.add, [st['Fb'], self.lbb], [st['Fb']])
            ACT(c, st['E'], st['F'], AF.Ln, [st['Fb']], [st['Eb']])
            TS(c, st['K'], st['F'], -1.0, 1.0, ALU.mult, ALU.add, [st['Fb']], [st['Kb']])
            c.P.op('dve', lambda e, st=st: e.tensor_tensor_scan(out=st['B'], data0=self.smask, data1=st['E'], initial=0.0,
                                                                 op0=ALU.mult, op1=ALU.add),
                   [st['Eb'], self.smb], [st['Bb']])
            NB = T // 16
            B3 = st['B'].rearrange("p (b l) -> p b l", l=16)
            D3 = st['D'].rearrange("p (b l) -> p b l", l=16)
            bmid = B3[:, :, 8:9].broadcast_to([128, NB, 16])
            blast = B3[:, :, 15:16].broadcast_to([128, NB, 16])
            TT(c, D3, B3, bmid, ALU.subtract, [st['Bb']], [st['Db']])
            ACT(c, st['E'], st['D'], AF.Exp, [st['Db']], [st['Eb']])
            TT(c, st['QR'], st['Q'], st['E'], ALU.mult, [st['Qb'], st['Eb']], [st['QRb']])
            ACT(c, st['E'], st['D'], AF.Exp, [st['Db']], [st['Eb']], scale=-1.0)
            TT(c, st['KR'], st['K'], st['E'], ALU.mult, [st['Kb'], st['Eb']], [st['KRb']])
            ACT(c, st['E'], st['B'], AF.Exp, [st['Bb']], [st['Eb']])
            TT(c, st['QI'], st['Q'], st['E'], ALU.mult, [st['Qb'], st['Eb']], [st['QIb']])
            TT(c, D3, blast, B3, ALU.subtract, [st['Bb']], [st['Db']])
            ACT(c, st['E'], st['D'], AF.Exp, [st['Db']], [st['Eb']])
            TT(c, st['KS'], st['K'], st['E'], ALU.mult, [st['Kb'], st['Eb']], [st['KSb']])
            ACT(c, st['DEC'], st['B'].rearrange("p (b l) -> p b l", l=16)[:, :, 15], AF.Exp, [st['Bb']], [st['DECb']])
            for g in range(G):
                gs = slice(g * 128, (g + 1) * 128)
                vh = self.Vt[:, g, hd * 128:(hd + 1) * 128]
                pa, pab = self.nq()
                MM(c, pa, st['KR'][:, gs], st['QR'][:, gs], True, True, [st['KRb'], st['QRb']], [pab])
                TT(c, st['AT'], pa, c.cst[:, C_HMASK:C_HMASK + 128], ALU.mult, [pab, c.cstb], [st['ATb']])
                po, pob = self.nq()
                MM(c, po, vh, st['AT'], True, True, [self.Vtb[g], st['ATb']], [pob])
                ACT(c, st['O'][:, gs], po, AF.Copy, [pob], [st['Ob']])
                pt, ptb = self.nq()
                TR(c, pt, st['KS'][:, gs], c.identf, [st['KSb'], c.cstb], [ptb])
                ACT(c, st['KSt'], pt, AF.Copy, [ptb], [st['KStb']])
                for j in range(8):
                    TS(c, st['KSm'][:, j, :], st['KSt'], c.cst[:, C_BLKM + j:C_BLKM + j + 1], None, ALU.mult, None,
                       [st['KStb'], c.cstb], [st['KSmb'][j]], eng='pool')
                bk = 6 + (self.gi % 2)
                self.gi += 1
                pi, pib = c.ps[bk][:, 0:128], c.psb[bk]
                for j in range(8):
                    blk = g * 8 + j
                    MM(c, pi[:, j * 16:(j + 1) * 16], Sv, st['QI'][:, blk * 16:(blk + 1) * 16], True, True, [Sb, st['QIb']], [pib])
                    pu, pub = self.nq()
                    MM(c, pu, st['KSm'][:, j, :], vh, True, True, [st['KSmb'][j], self.Vtb[g]], [pub])
                    STT(c, Sv, Sv, st['DEC'][:, blk:blk + 1], pu, ALU.mult, ALU.add, [Sb, st['DECb'], pub], [Sb])
                TT(c, st['O'][:, gs], st['O'][:, gs], pi, ALU.add, [pib, st['Ob']], [st['Ob']])
            ACT(c, st['SQh'], st['O'], AF.Square, [st['Ob']], [st['SQhb']])
            ps, psb = c.next_ps()
            MM(c, ps[:, 0:T], c.ones_h, st['SQh'], True, True, [st['SQhb'], c.miscb], [psb])
            rstd_from_ps(c, ps, psb, st['D'], st['Db'])
            STT(c, st['O'], st['O'], gn, st['D'], ALU.mult, ALU.mult, [st['Ob'], st['Db'], c.cstb], [st['Ob']])
            TT(c, self.YV[:, hd, :], st['O'], st['Gt'], ALU.mult, [st['Ob'], st['Gtb']], [self.YVb[hd]])
        out_proj_residual(c, self.wout, self.YV, self.YVb, self.gpost, self.Y, self.Yflat, self.Yb, self.rstd, self.rstdb)


def extra_inputs(S, T):
    NR = T // 128
    tab = np.zeros((128, (NR + 1) * T), np.float32)
    kk = np.arange(128)[:, None].astype(np.float64)
    qq = np.arange(T)[None, :].astype(np.float64)
    tab[:, 0:T] = kk - qq
    for r in range(NR):
        kpos = 128 * r + kk
        allowed = (kpos // 64) <= (qq // 64)
        tab[:, (r + 1) * T:(r + 2) * T] = np.where(allowed, -np.abs(qq - kpos), -1.0e6)
    return {"atab": tab}


class AttnSub(MixBase):
    def __init__(self, c, L, layer):
        self.common(c, layer)
        T = c.T; R = c.R; S = c.S
        self.layer = layer
        W0 = c.A_W
        self.win = load_slots(c, L['m_in'], W0, 8192, lambda v: v.rearrange("p (k n) -> p k n", n=512), 'awi')
        self.wout = load_slots(c, L['m_out'], W0 + 49152, 2048, lambda v: v.rearrange("p (k n) -> p k n", n=128), 'awo')
        o = W0 + 65536
        self.Kb = []; self.Kbb = []; self.Vb = []; self.Vbb = []; self.Kst = []; self.Kstb = []
        for i in range(2):
            self.Kb.append(c.vb(o, S)); self.Kbb.append(R.alloc(o, 2 * S, f'Kb{i}')); o += 2 * S
            self.Vb.append(c.vb(o, S).rearrange("p (t n) -> p t n", n=128)); self.Vbb.append(R.alloc(o, 2 * S, f'Vb{i}')); o += 2 * S
        for i in range(2):
            self.Kst.append(c.vb(o, T)); self.Kstb.append(R.alloc(o, 2 * T, f'Kst{i}')); o += 2 * T
        self.sm = c.vf(o, 80); self.smb = R.alloc(o, 320, 'attn_small'); o += 320
        assert o <= c.A_W + c.WBYTES, o - c.A_W
        lam_init = 0.8 - 0.6 * math.exp(-0.3 * layer)
        lamv = c.cst[:, C_LAM:C_LAM + 256]
        pr = self.sm[:, 0:64]; s1 = self.sm[:, 64:65]; s2 = self.sm[:, 65:66]
        self.nlam = self.sm[:, 66:67]; self.subl = self.sm[:, 67:68]
        TT(c, pr, lamv[:, 0:64], lamv[:, 64:128], ALU.mult, [c.cstb], [self.smb])
        c.P.op('dve', lambda e: e.tensor_reduce(out=s1, in_=pr, axis=AX.X, op=ALU.add), [self.smb], [self.smb])
        TT(c, pr, lamv[:, 128:192], lamv[:, 192:256], ALU.mult, [c.cstb, self.smb], [self.smb])
        c.P.op('dve', lambda e: e.tensor_reduce(out=s2, in_=pr, axis=AX.X, op=ALU.add), [self.smb], [self.smb])
        ACT(c, s1, s1, AF.Exp, [self.smb], [self.smb])
        ACT(c, s2, s2, AF.Exp, [self.smb], [self.smb])
        TT(c, s1, s2, s1, ALU.subtract, [self.smb], [self.smb])
        TS(c, self.nlam, s1, -lam_init, None, ALU.add, None, [self.smb], [self.smb])
        TS(c, self.subl, c.cst[:, C_SUBLN:C_SUBLN + 1], 1.0 - lam_init, None, ALU.mult, None, [c.cstb, self.smb], [self.smb])
        NR = T // 128
        self.NR = NR
        ob = c.A_BS + 16 * T
        self.tab = c.vf(ob, (NR + 1) * T)
        self.tabb = R.alloc(ob, 4 * T * (NR + 1), 'atab')
        DMA(c, 'sp', self.tab, c.atab[:, :], [], [self.tabb])
        ob += 4 * T * (NR + 1)
        self.tmp = []; self.tmpb = []
        for i in range(2):
            self.tmp.append(c.vf(ob, T)); self.tmpb.append(R.alloc(ob, 4 * T, f'atmp{i}')); ob += 4 * T
        assert ob <= c.A_BS + 44 * T, (ob - c.A_BS, 44 * T)
        of = c.A_FS + 36 * T
        self.Qb = []; self.Qbb = []; self.PT = []; self.PTb = []
        for i in range(2):
            self.Qb.append(c.vb(of, T)); self.Qbb.append(R.alloc(of, 2 * T, f'Qb{i}')); of += 2 * T
        for i in range(2):
            self.PT.append(c.vb(of, T)); self.PTb.append(R.alloc(of, 2 * T, f'PT{i}')); of += 2 * T
        assert of <= c.A_FS + 48 * T
        self.ones_k = c.vb(c.A_MISC + 1024, 128)
        MEMSET(c, self.ones_k, 1.0, [c.miscb])
        self.Kd = [Buf(f'ktd{h}') for h in range(8)]
        self.Vd = [Buf(f'vd{h}') for h in range(8)]
        self.pi = 0

    def tile(self, ti):
        c = self.c; T = c.T; S = c.S; NR = self.NR
        G = T // 128
        t0 = ti * T
        c.ps_base = 4; c.ps_lim = 4
        prenorm(c, self.gpre, self.SQ, self.SQb, self.rstd, self.rstdb)
        for hd in range(8):
            wv, wb = self.win[2 + hd // 4]
            hs = (hd % 4) * 128
            ps, psb = c.next_ps()
            for k in range(8):
                MM(c, ps[:, 0:T], wv[:, k, hs:hs + 128], c.XN[:, k, :], k == 0, k == 7, [wb, c.XNb], [psb])
            ks = self.Kst[hd % 2]; ksb = self.Kstb[hd % 2]
            ACT(c, ks, ps[:, 0:T], AF.Copy, [psb], [ksb])
            DMA(c, 'sp', c.ktd[hd, :, t0:t0 + T], ks, [ksb], [self.Kd[hd]])
        Vst = c.vb(c.A_BS, 8 * T).rearrange("p (g n) -> p g n", n=1024)
        for g in range(G):
            for half in range(2):
                wv, wb = self.win[4 + half]
                ps, psb = c.next_ps()
                for k in range(8):
                    MM(c, ps[:, 0:512], c.XN[:, k, g * 128:(g + 1) * 128], wv[:, k, :], k == 0, k == 7, [wb, c.XNb], [psb])
                ACT(c, Vst[:, g, half * 512:(half + 1) * 512], ps[:, 0:512], AF.Copy, [psb], self.YVb)
        for hd in range(8):
            DMA(c, 'sp', c.vd[hd, :, ti * G:(ti + 1) * G, :], Vst[:, :, hd * 128:(hd + 1) * 128], self.YVb, [self.Vd[hd]])
        nk = (ti + 1) * T
        nkt = nk // 128
        scale = 0.125
        for hd in range(8):
            par = hd % 2
            slope = 2.0 ** (-(hd + 1))
            Kb = self.Kb[par]; Kbb = self.Kbb[par]; Vb = self.Vb[par]; Vbb = self.Vbb[par]
            DMA(c, 'sp', Kb[:, 0:nk], c.ktd[hd, :, 0:nk], [self.Kd[hd]], [Kbb])
            DMA(c, 'sp', Vb[:, 0:nkt, :], c.vd[hd, :, 0:nkt, :], [self.Vd[hd]], [Vbb])
            wv, wb = self.win[hd // 4]
            hs = (hd % 4) * 128
            ps, psb = c.next_ps()
            for k in range(8):
                MM(c, ps[:, 0:T], wv[:, k, hs:hs + 128], c.XN[:, k, :], k == 0, k == 7, [wb, c.XNb], [psb])
            Qb = self.Qb[par]; Qbb = self.Qbb[par]
            ACT(c, Qb, ps[:, 0:T], AF.Copy, [psb], [Qbb])
            for g2 in range(2):
                oacc, oaccb = c.ps[2 * g2], c.psb[2 * g2]
                dacc, daccb = c.ps[2 * g2 + 1], c.psb[2 * g2 + 1]
                rows = slice(g2 * 64, (g2 + 1) * 64)
                for kt in range(nkt):
                    if kt < ti * G:
                        c0 = 0; tb = self.tab[:, 0:T]; delta = float(t0 - 128 * kt)
                    else:
                        r = kt - ti * G
                        c0 = 128 * r; tb = self.tab[:, (r + 1) * T:(r + 2) * T]; delta = 0.0
                    ps, psb = c.next_ps()
                    MM(c, ps[:, c0:T], Kb[rows, kt * 128:(kt + 1) * 128], Qb[rows, c0:T], True, True, [Kbb, Qbb], [psb])
                    i = self.pi % 2
                    self.pi += 1
                    tmp = self.tmp[i]; tmpb = self.tmpb[i]; PT = self.PT[i]; PTb = self.PTb[i]
                    STT(c, tmp[:, c0:T], ps[:, c0:T], scale / slope, tb[:, c0:T], ALU.mult, ALU.add, [psb, self.tabb], [tmpb])
                    ACT(c, PT[:, c0:T], tmp[:, c0:T], AF.Exp, [tmpb], [PTb], bias=self.biasc(-slope * delta), scale=slope)
                    MM(c, oacc[:, c0:T], Vb[:, kt, :], PT[:, c0:T], kt == 0, kt == nkt - 1, [Vbb, PTb], [oaccb])
                    MM(c, dacc[:, c0:T], self.ones_k, PT[:, c0:T], kt == 0, kt == nkt - 1, [c.miscb, PTb], [daccb])
            t0v, t0b, t1v, t1b = self.tmp[0], self.tmpb[0], self.tmp[1], self.tmpb[1]
            RECIP(c, t0v, c.ps[1][:, 0:T], [c.psb[1]], [t0b])
            TT(c, t0v, c.ps[0][:, 0:T], t0v, ALU.mult, [c.psb[0], t0b], [t0b])
            RECIP(c, t1v, c.ps[3][:, 0:T], [c.psb[3]], [t1b])
            TT(c, t1v, c.ps[2][:, 0:T], t1v, ALU.mult, [c.psb[2], t1b], [t1b])
            STT(c, t0v, t1v, self.nlam, t0v, ALU.mult, ALU.add, [t0b, t1b, self.smb], [t0b])
            sq = self.PT[0]; sqb = self.PTb[0]
            ACT(c, sq, t0v, AF.Square, [t0b], [sqb])
            ps, psb = c.next_ps()
            MM(c, ps[:, 0:T], c.ones_h, sq, True, True, [sqb, c.miscb], [psb])
            rstd_from_ps(c, ps, psb, t1v, t1b)
            STT(c, self.YV[:, hd, :], t0v, self.subl, t1v, ALU.mult, ALU.mult, [t0b, t1b, self.smb], [self.YVb[hd]])
        c.ps_base = 0; c.ps_lim = 8
        out_proj_residual(c, self.wout, self.YV, self.YVb, self.gpost, self.Y, self.Yflat, self.Yb, self.rstd, self.rstdb)

    def biasc(self, v):
        return float(v)


SEQ = 8192
TILE = 512
ALL_SUBLAYERS = [(l, k) for l in range(DEPTH) for k in ('f1', 'mix', 'f2')]


def kernel(x, norm_gains, ffn1_w_in, ffn1_w_out, ffn2_w_in, ffn2_w_out,
           hgrn_w_in, hgrn_gnorm, hgrn_w_out, hgrn_lb_raw,
           diff_w_in, diff_lambda, diff_subln, diff_w_out,
           pool_w_in, pool_w_group, pool_scale, pool_w_out):
    inp = dict(x=x, norm_gains=norm_gains, ffn1_w_in=ffn1_w_in, ffn1_w_out=ffn1_w_out,
               ffn2_w_in=ffn2_w_in, ffn2_w_out=ffn2_w_out, hgrn_w_in=hgrn_w_in, hgrn_gnorm=hgrn_gnorm,
               hgrn_w_out=hgrn_w_out, hgrn_lb_raw=hgrn_lb_raw, diff_w_in=diff_w_in, diff_lambda=diff_lambda,
               diff_subln=diff_subln, diff_w_out=diff_w_out, pool_w_in=pool_w_in, pool_w_group=pool_w_group,
               pool_scale=pool_scale, pool_w_out=pool_w_out)
    inp = {k: np.asarray(v, dtype=np.float32) for k, v in inp.items()}
    B, S, _ = inp['x'].shape
    assert S == SEQ and B == 8
    ws, plan = plan_weights(inp)
    wst = ws.array()
    cst = pack_consts(inp)
    ext = extra_inputs(S, TILE)
    nc = build(S, TILE, plan, wst.shape[1], ALL_SUBLAYERS)
    in_maps = []
    for b in range(B):
        in_maps.append({"xT": np.ascontiguousarray(inp['x'][b].T), "wst": wst, "cst": cst, "atab": ext["atab"]})
    res = run_bass_kernel_spmd(nc, in_maps, core_ids=list(range(B)))
    out = np.empty((B, S, D), np.float32)
    for b in range(B):
        out[b] = res.results[b]["oT"].T
    return out
```

```python
import math
import numpy as np
import concourse.bass as bass
import concourse.mybir as mybir
from concourse.bass_utils import run_bass_kernel_spmd

F32 = mybir.dt.float32
BF16 = mybir.dt.bfloat16
AF = mybir.ActivationFunctionType
ALU = mybir.AluOpType
AX = mybir.AxisListType

D = 1024
NC8 = 8
DFF = 2816
NJ = 22
DEPTH = 4
EPS = 1e-6
ENGS = ['pe', 'act', 'dve', 'pool', 'sp']
SAME_SYNC = {'pe': False, 'act': True, 'dve': True, 'pool': True, 'sp': True}
EPOCH = 30000
NDSEM = 16


class Op:
    __slots__ = ('eng', 'fn', 'deps', 'dma', 'dsem', 'dval', 'signal', 'count', 'idx')


class Buf:
    __slots__ = ('name', 'w', 'r', 'lo', 'hi')

    def __init__(self, name=''):
        self.name = name
        self.w = {}
        self.r = {}


def _key(o):
    return ('d', o.dsem) if o.dma else o.eng


def _ord(o):
    return o.dval if o.dma else o.idx


class Prog:
    def __init__(self, nc):
        self.nc = nc
        self.ops = {e: [] for e in ENGS}
        self.dsem_val = [0] * NDSEM
        self.dsem_last = [None] * NDSEM
        self.next_dsem = {'sp': 0, 'pool': 0, 'act': 0}

    def op(self, eng, fn, reads=(), writes=(), dma=False):
        o = Op()
        o.eng = eng; o.fn = fn; o.dma = dma; o.signal = False; o.count = None
        o.idx = len(self.ops[eng]); o.dsem = None; o.dval = None
        deps = {}

        def add(d):
            k = _key(d)
            c = deps.get(k)
            if c is None or _ord(d) > _ord(c):
                deps[k] = d
        for b in reads:
            for d in b.w.values():
                add(d)
        for b in writes:
            for d in b.w.values():
                add(d)
            for d in b.r.values():
                add(d)
        if dma:
            half = NDSEM // 2
            base = 0 if eng == 'sp' else half
            s = base + self.next_dsem[eng]
            self.next_dsem[eng] = (self.next_dsem[eng] + 1) % half
            if self.dsem_last[s] is not None:
                add(self.dsem_last[s])
            o.dsem = s
            self.dsem_val[s] += 16
            o.dval = self.dsem_val[s]
            self.dsem_last[s] = o
        o.deps = [d for d in deps.values() if d.dma or d.eng != eng or SAME_SYNC[eng]]
        for d in o.deps:
            if not d.dma:
                d.signal = True
        k = _key(o)
        for b in reads:
            b.r[k] = o
        for b in writes:
            b.w = {k: o}
            b.r = {}
        self.ops[eng].append(o)
        return o

    def emit(self):
        nc = self.nc
        nsig = {}
        for e in ENGS:
            c = 0
            for o in self.ops[e]:
                if o.signal:
                    c += 1
                    o.count = c
            nsig[e] = c
        esems = {e: [nc.alloc_semaphore(f"s_{e}_{i}") for i in range(max(1, (nsig[e] + EPOCH - 1) // EPOCH))]
                 for e in ENGS}
        dsems = [nc.alloc_semaphore(f"s_dma_{i}") for i in range(NDSEM)]

        def semval(d):
            if d.dma:
                return ('d', d.dsem), dsems[d.dsem], d.dval
            ep = (d.count - 1) // EPOCH
            return (d.eng, ep), esems[d.eng][ep], d.count - ep * EPOCH

        ops = self.ops
        dsem_val = self.dsem_val

        def run(eng, e):
            waited = {}
            for o in ops[eng]:
                for d in o.deps:
                    k, s, v = semval(d)
                    if waited.get(k, 0) >= v:
                        continue
                    waited[k] = v
                    e.wait_ge(s, v)
                ins = o.fn(e)
                if o.dma:
                    ins.then_inc(dsems[o.dsem], 16)
                elif o.signal:
                    _, s, _ = semval(o)
                    ins.then_inc(s, 1)
            if eng == 'sp':
                for i in range(NDSEM):
                    if dsem_val[i] > 0:
                        e.wait_ge(dsems[i], dsem_val[i])

        with nc.Block() as block:
            @block.tensor
            def _(e):
                run('pe', e)

            @block.scalar
            def _(e):
                run('act', e)

            @block.vector
            def _(e):
                run('dve', e)

            @block.gpsimd
            def _(e):
                run('pool', e)

            @block.sync
            def _(e):
                run('sp', e)


class Region:
    def __init__(self):
        self.bufs = []

    def alloc(self, lo, n, name=''):
        b = Buf(name)
        b.lo = lo; b.hi = lo + n
        for o in self.bufs:
            if o.lo < b.hi and b.lo < o.hi:
                for k, d in o.w.items():
                    c = b.w.get(k)
                    if c is None or _ord(d) > _ord(c):
                        b.w[k] = d
                for k, d in o.r.items():
                    c = b.r.get(k)
                    if c is None or _ord(d) > _ord(c):
                        b.r[k] = d
        self.bufs.append(b)
        return b


def pack_cols(W, col_groups):
    K = W.shape[0]
    KC = K // 128
    Wr = W.reshape(KC, 128, W.shape[1])
    outs = []
    for cols in col_groups:
        blk = Wr[:, :, cols]
        outs.append(np.ascontiguousarray(blk.transpose(1, 0, 2)).reshape(128, -1))
    return outs


class WStream:
    def __init__(self):
        self.parts = []
        self.off = 0

    def add(self, arr):
        o = self.off
        self.parts.append(arr)
        self.off += arr.shape[1]
        return (o, arr.shape[1])

    def array(self):
        return np.ascontiguousarray(np.concatenate(self.parts, axis=1).astype(np.float32))


def ffn_groups():
    gs = []
    for s in range(11):
        c0 = s * 256
        gs.append(np.concatenate([np.arange(c0, c0 + 256), DFF + np.arange(c0, c0 + 256)]))
    return gs


def out_groups(n_out=D):
    return [np.arange(m * 128, (m + 1) * 128) for m in range(n_out // 128)]


def plan_weights(inputs):
    ws = WStream()
    plan = []
    for i in range(DEPTH):
        kind, j = i % 3, i // 3
        L = {}
        for nm, win, wout in (('f1', 'ffn1_w_in', 'ffn1_w_out'), ('f2', 'ffn2_w_in', 'ffn2_w_out')):
            L[nm + '_in'] = [ws.add(a) for a in pack_cols(inputs[win][i], ffn_groups())]
            L[nm + '_out'] = [ws.add(a) for a in pack_cols(inputs[wout][i], out_groups())]
        if kind == 0:
            W = inputs['hgrn_w_in'][j]
            L['m_in'] = [ws.add(a) for a in pack_cols(W, [np.arange(c * 512, (c + 1) * 512) for c in range(8)])]
            L['m_out'] = [ws.add(a) for a in pack_cols(inputs['hgrn_w_out'][j], out_groups())]
        elif kind == 1:
            W = inputs['diff_w_in'][j]
            L['m_in'] = [ws.add(a) for a in pack_cols(W, [np.arange(c * 512, (c + 1) * 512) for c in range(6)])]
            L['m_out'] = [ws.add(a) for a in pack_cols(inputs['diff_w_out'][j], out_groups())]
        else:
            L['m_in'] = [ws.add(a) for a in pack_cols(inputs['pool_w_in'][j], [np.arange(c * 512, (c + 1) * 512) for c in range(2)])]
            wg = inputs['pool_w_group'][j]
            L['m_grp'] = [ws.add(pack_cols(wg[g], [np.arange(256)])[0]) for g in range(4)]
            L['m_out'] = [ws.add(a) for a in pack_cols(inputs['pool_w_out'][j], out_groups())]
        plan.append(L)
    return ws, plan


C_GAIN = 0
C_GNORM = 192
C_SUBLN = 194
C_PSCALE = 195
C_LBRAW = 203
C_LAM = 235
C_IDENT = 491
C_HMASK = 619
C_PINV = 747
C_BLKM = 811
NCST = 819


def pack_consts(inp):
    c = np.zeros((128, NCST), np.float32)
    c[:, C_GAIN:C_GAIN + 192] = inp['norm_gains'].reshape(4, 6, 8, 128).transpose(3, 0, 1, 2).reshape(128, 192)
    c[:, C_GNORM:C_GNORM + 2] = inp['hgrn_gnorm'].T
    c[:, C_SUBLN:C_SUBLN + 1] = inp['diff_subln'].T
    c[:, C_PSCALE:C_PSCALE + 8] = inp['pool_scale'][0].reshape(8, 128).T
    c[:, C_LBRAW:C_LBRAW + 32] = inp['hgrn_lb_raw'].reshape(4, 8, 128).transpose(2, 1, 0).reshape(128, 32)
    c[:, C_LAM:C_LAM + 256] = np.broadcast_to(inp['diff_lambda'][0].reshape(1, 256), (128, 256))
    c[:, C_IDENT:C_IDENT + 128] = np.eye(128, dtype=np.float32)
    s = np.arange(128)[:, None]; t = np.arange(128)[None, :]
    c[:, C_HMASK:C_HMASK + 128] = ((s // 16 == t // 16) & (s <= t)).astype(np.float32)
    pinv = np.zeros((4, 16), np.float32)
    for g, w in enumerate((2, 4, 8, 16)):
        pinv[g] = 1.0 / np.minimum(np.arange(16) + 1, w)
    c[:, C_PINV:C_PINV + 64] = np.broadcast_to(pinv.reshape(1, 64), (128, 64))
    c[:, C_BLKM:C_BLKM + 8] = (np.arange(128)[:, None] // 16 == np.arange(8)[None, :]).astype(np.float32)
    return c


class Ctx:
    pass


def build(S, T, plan, wtot, sublayers, first_src_is_x=True):
    nc = bass.Bass("TRN2", target_bir_lowering=False)
    P = Prog(nc)
    NT = S // T
    xT = nc.dram_tensor("xT", [D, S], F32, kind="ExternalInput").ap()
    wst = nc.dram_tensor("wst", [128, wtot], F32, kind="ExternalInput").ap()
    cstd = nc.dram_tensor("cst", [128, NCST], F32, kind="ExternalInput").ap()
    oT = nc.dram_tensor("oT", [D, S], F32, kind="ExternalOutput").ap()
    hscr = nc.dram_tensor("hscr", [D, S], F32).ap()
    NR = T // 128
    atab = nc.dram_tensor("atab", [128, (NR + 1) * T], F32, kind="ExternalInput").ap()
    ktd = nc.dram_tensor("ktd", [8, 128, S], BF16).ap()
    vd = nc.dram_tensor("vd", [8, 128, S // 128, 128], BF16).ap()

    A_CST = 0
    A_MISC = 3328
    A_H = A_MISC + 1280
    A_XN = A_H + 32 * T
    A_FS = A_XN + 16 * T
    A_BS = A_FS + 48 * T
    A_W = A_BS + 44 * T
    WBYTES = 135168
    A_END = A_W + WBYTES
    arena = nc.alloc_sbuf_tensor("arena", [128, A_END // 2], BF16)
    R = Region()

    def vb(off, n):
        return arena[:, off // 2: off // 2 + n]

    def vf(off, n):
        return arena[:, off // 2: off // 2 + 2 * n].bitcast(F32)

    c = Ctx()
    c.nc = nc; c.P = P; c.R = R; c.T = T; c.S = S; c.NT = NT
    c.vb = vb; c.vf = vf; c.wst = wst
    c.atab = atab; c.ktd = ktd; c.vd = vd
    c.A_MISC = A_MISC; c.A_W = A_W; c.A_FS = A_FS; c.A_BS = A_BS; c.A_XN = A_XN; c.A_H = A_H; c.WBYTES = WBYTES
    cst = vf(A_CST, NCST)
    c.cst = cst
    cstb = R.alloc(A_CST, NCST * 4, 'cst')
    c.cstb = cstb
    c.P = P
    DMA(c, 'sp', cst, cstd[:, :], [], [cstb])
    ones_d = vb(A_MISC, 128); ones_h = vb(A_MISC + 256, 128); identb = vb(A_MISC + 512, 128)
    miscb = R.alloc(A_MISC, 1280, "misc")
    c.ones_d = ones_d; c.ones_h = ones_h; c.identb = identb; c.miscb = miscb
    c.identf = cst[:, C_IDENT:C_IDENT + 128]
    c.epsc = vf(A_MISC + 768, 1)
    MEMSET(c, c.epsc, EPS, [miscb])
    MEMSET(c, ones_d, 1.0 / D, [miscb])
    MEMSET(c, ones_h, 1.0 / 128, [miscb])
    CP(c, identb, cst[:, C_IDENT:C_IDENT + 128], [cstb], [miscb])

    c.ps = [nc.alloc_psum_tensor(f"ps{i}", [128, 512], F32) for i in range(8)]
    c.PR = Region()
    c.psi = 0
    c.ps_lim = 8

    def reset_ps():
        c.psb = [c.PR.alloc(i * 2048, 2048, f"ps{i}") for i in range(8)]
        c.ps_lim = 8
        c.psi = 0
        c.ps_base = 0
    c.reset_ps = reset_ps
    reset_ps()

    c.ps_base = 0

    def next_ps():
        i = c.psi % c.ps_lim
        c.psi = (i + 1) % c.ps_lim
        return c.ps[c.ps_base + i], c.psb[c.ps_base + i]
    c.next_ps = next_ps

    c.H = vf(A_H, 8 * T).rearrange("p (c t) -> p c t", t=T)
    c.Hflat = vf(A_H, 8 * T)
    c.Hb = R.alloc(A_H, 32 * T, 'H')
    c.XN = vb(A_XN, 8 * T).rearrange("p (c t) -> p c t", t=T)
    c.XNflat = vb(A_XN, 8 * T)
    c.XNb = R.alloc(A_XN, 16 * T, 'XN')

    dt = {}

    def dtile(name, i):
        k = (name, i)
        if k not in dt:
            dt[k] = Buf(f"{name}{i}")
        return dt[k]

    tens = {'x': xT, 'h': hscr, 'o': oT}
    nsl = len(sublayers)
    c.state = {}
    for si, (layer, kind) in enumerate(sublayers):
        src = 'x' if si == 0 else 'h'
        dst = 'o' if si == nsl - 1 else 'h'
        L = plan[layer]
        mk = layer % 3
        c.reset_ps()
        if kind in ('f1', 'f2'):
            sub = FFNSub(c, L[kind + '_in'], L[kind + '_out'], layer, 0 if kind == 'f1' else 4)
        elif mk == 2:
            sub = PoolSub(c, L, layer)
        elif mk == 0:
            sub = HgrnSub(c, L, layer)
        else:
            sub = AttnSub(c, L, layer)
        for ti in range(NT):
            t0 = ti * T
            sa = tens[src].rearrange("(c p) s -> p c s", p=128)[:, :, t0:t0 + T]
            DMA(c, 'sp', c.H, sa, [dtile(src, ti)], [c.Hb])
            sub.tile(ti)
            da = tens[dst].rearrange("(c p) s -> p c s", p=128)[:, :, t0:t0 + T]
            DMA(c, 'sp', da, c.H, [c.Hb], [dtile(dst, ti)])
    P.emit()
    return nc


def MM(c, out, lhsT, rhs, start, stop, reads, writes):
    return c.P.op('pe', lambda e: e.matmul(out, lhsT, rhs, start=start, stop=stop), reads, writes)


def TR(c, out, in_, ident, reads, writes):
    return c.P.op('pe', lambda e: e.transpose(out, in_, ident), reads, writes)


def ACT(c, out, in_, func, reads, writes, bias=0.0, scale=1.0):
    return c.P.op('act', lambda e: e.activation(out=out, in_=in_, func=func, bias=bias, scale=scale), reads, writes)


def TT(c, out, in0, in1, op, reads, writes, eng='dve'):
    return c.P.op(eng, lambda e: e.tensor_tensor(out=out, in0=in0, in1=in1, op=op), reads, writes)


def STT(c, out, in0, scalar, in1, op0, op1, reads, writes, eng='dve'):
    return c.P.op(eng, lambda e: e.scalar_tensor_tensor(out=out, in0=in0, scalar=scalar, in1=in1, op0=op0, op1=op1), reads, writes)


def TS(c, out, in0, s1, s2, op0, op1, reads, writes, eng='dve'):
    if s2 is None:
        return c.P.op(eng, lambda e: e.tensor_scalar(out=out, in0=in0, scalar1=s1, scalar2=None, op0=op0), reads, writes)
    return c.P.op(eng, lambda e: e.tensor_scalar(out=out, in0=in0, scalar1=s1, scalar2=s2, op0=op0, op1=op1), reads, writes)


def CP(c, out, in_, reads, writes, eng='dve'):
    return c.P.op(eng, lambda e: e.tensor_copy(out=out, in_=in_), reads, writes)


def RECIP(c, out, in_, reads, writes):
    return c.P.op('dve', lambda e: e.reciprocal(out=out, in_=in_), reads, writes)


def MEMSET(c, out, val, writes, eng='pool'):
    return c.P.op(eng, lambda e: e.memset(out, val), (), writes)


def DMA(c, eng, out, in_, reads, writes):
    return c.P.op(eng, lambda e: e.dma_start(out=out, in_=in_), reads, writes, dma=True)


def load_slots(c, slots, base, stride_bytes, shape_fn, name):
    out = []
    for i, (off, n) in enumerate(slots):
        lo = base + i * stride_bytes
        assert n * 2 <= stride_bytes
        assert lo + n * 2 <= c.A_W + c.WBYTES, (name, i)
        b = c.R.alloc(lo, n * 2, f"{name}{i}")
        v = c.vb(lo, n)
        DMA(c, 'pool', v, c.wst[:, off:off + n], [], [b])
        out.append((shape_fn(v), b))
    return out


def rstd_from_ps(c, ps, psb, rstd_view, rstd_buf, n=None):
    n = c.T if n is None else n
    ACT(c, rstd_view, ps[:, 0:n], AF.Sqrt, [psb, c.miscb], [rstd_buf], bias=c.epsc)
    RECIP(c, rstd_view, rstd_view, [rstd_buf], [rstd_buf])


def rms_stats(c, sq_view, sq_bufs, nchunks, ones, rstd_view, rstd_buf):
    T = c.T
    ps, psb = c.next_ps()
    for k in range(nchunks):
        MM(c, ps[:, 0:T], ones, sq_view[:, k, :], k == 0, k == nchunks - 1, list(sq_bufs) + [c.miscb], [psb])
    rstd_from_ps(c, ps, psb, rstd_view, rstd_buf)


def prenorm(c, gcol, sq_view, sq_bufs, rstd_view, rstd_buf):
    ACT(c, sq_view, c.H, AF.Square, [c.Hb], list(sq_bufs))
    rms_stats(c, sq_view, sq_bufs, 8, c.ones_d, rstd_view, rstd_buf)
    for k in range(8):
        g = c.cst[:, gcol + k: gcol + k + 1]
        STT(c, c.XN[:, k, :], c.H[:, k, :], g, rstd_view, ALU.mult, ALU.mult, [c.Hb, rstd_buf, c.cstb], [c.XNb])


def postnorm_residual(c, gcol, Y, Yflat, Yb, sq_view, sq_bufs, rstd_view, rstd_buf, factor):
    rms_stats(c, sq_view, sq_bufs, 8, c.ones_d, rstd_view, rstd_buf)
    for k in range(8):
        g = c.cst[:, gcol + k: gcol + k + 1]
        STT(c, Y[:, k, :], Y[:, k, :], g, rstd_view, ALU.mult, ALU.mult, [Yb, rstd_buf, c.cstb], [Yb])
    STT(c, c.Hflat, Yflat, float(factor), c.Hflat, ALU.mult, ALU.add, [Yb, c.Hb], [c.Hb])


class FFNSub:
    def __init__(self, c, slots_in, slots_out, layer, nbase):
        self.c = c
        T = c.T
        self.gpre = C_GAIN + (layer * 6 + nbase) * 8
        self.gpost = C_GAIN + (layer * 6 + nbase + 1) * 8
        self.win = load_slots(c, slots_in, c.A_W, 8192, lambda v: v.rearrange("p (k n) -> p k n", n=512), 'wi')
        self.wout = load_slots(c, slots_out, c.A_W + 11 * 8192, 5632, lambda v: v.rearrange("p (k n) -> p k n", n=128), 'wo')
        R = c.R
        self.Y = c.vf(c.A_FS, 8 * T).rearrange("p (c t) -> p c t", t=T)
        self.Yflat = c.vf(c.A_FS, 8 * T)
        self.Yb = R.alloc(c.A_FS, 32 * T, 'Y')
        self.rstd = c.vf(c.A_FS + 32 * T, T)
        self.rstdb = R.alloc(c.A_FS + 32 * T, 4 * T, 'rstd')
        self.sg = [c.vf(c.A_FS + 36 * T + 4 * T * i, T) for i in range(2)]
        self.sgb = [R.alloc(c.A_FS + 36 * T + 4 * T * i, 4 * T, f'sg{i}') for i in range(2)]
        self.hid = c.vb(c.A_BS, NJ * T).rearrange("p (c t) -> p c t", t=T)
        self.hidb = [R.alloc(c.A_BS + 2 * T * j, 2 * T, f'hid{j}') for j in range(NJ)]

    def tile(self, ti):
        c = self.c; T = c.T
        prenorm(c, self.gpre, self.hid[:, 0:8, :], self.hidb[0:8], self.rstd, self.rstdb)
        for s in range(11):
            wv, wb = self.win[s]
            for jj in range(2):
                j = 2 * s + jj
                psg, psgb = c.next_ps()
                for k in range(8):
                    MM(c, psg[:, 0:T], wv[:, k, jj * 128:(jj + 1) * 128], c.XN[:, k, :], k == 0, k == 7, [wb, c.XNb], [psgb])
                psu, psub = c.next_ps()
                for k in range(8):
                    MM(c, psu[:, 0:T], wv[:, k, 256 + jj * 128:256 + (jj + 1) * 128], c.XN[:, k, :], k == 0, k == 7, [wb, c.XNb], [psub])
                sg = self.sg[j % 2]; sgb = self.sgb[j % 2]
                ACT(c, sg, psg[:, 0:T], AF.Silu, [psgb], [sgb])
                TT(c, self.hid[:, j, :], sg, psu[:, 0:T], ALU.mult, [sgb, psub], [self.hidb[j]])
        for m in range(8):
            wv, wb = self.wout[m]
            ps, psb = c.next_ps()
            for j in range(NJ):
                MM(c, ps[:, 0:T], wv[:, j, :], self.hid[:, j, :], j == 0, j == NJ - 1, [wb, self.hidb[j]], [psb])
            ACT(c, self.Y[:, m, :], ps[:, 0:T], AF.Copy, [psb], [self.Yb])
            ACT(c, c.XN[:, m, :], ps[:, 0:T], AF.Square, [psb], [c.XNb])
        postnorm_residual(c, self.gpost, self.Y, self.Yflat, self.Yb, c.XN, [c.XNb], self.rstd, self.rstdb, 0.5)


def out_proj_residual(c, wout, IN, INbufs, gpost, Y, Yflat, Yb, rstd, rstdb, factor=1.0):
    T = c.T
    for m in range(8):
        wv, wb = wout[m]
        ps, psb = c.next_ps()
        for k in range(8):
            MM(c, ps[:, 0:T], wv[:, k, :], IN[:, k, :], k == 0, k == 7, [wb] + list(INbufs), [psb])
        ACT(c, Y[:, m, :], ps[:, 0:T], AF.Copy, [psb], [Yb])
        ACT(c, c.XN[:, m, :], ps[:, 0:T], AF.Square, [psb], [c.XNb])
    postnorm_residual(c, gpost, Y, Yflat, Yb, c.XN, [c.XNb], rstd, rstdb, factor)


class MixBase:
    def common(self, c, layer):
        T = c.T; R = c.R
        self.c = c
        self.gpre = C_GAIN + (layer * 6 + 2) * 8
        self.gpost = C_GAIN + (layer * 6 + 3) * 8
        self.Y = c.vf(c.A_FS, 8 * T).rearrange("p (c t) -> p c t", t=T)
        self.Yflat = c.vf(c.A_FS, 8 * T)
        self.Yb = R.alloc(c.A_FS, 32 * T, 'Y')
        self.rstd = c.vf(c.A_FS + 32 * T, T)
        self.rstdb = R.alloc(c.A_FS + 32 * T, 4 * T, 'rstd')
        self.YV = c.vb(c.A_BS, 8 * T).rearrange("p (c t) -> p c t", t=T)
        self.YVb = [R.alloc(c.A_BS + 2 * T * k, 2 * T, f'YV{k}') for k in range(8)]
        self.SQ = self.YV
        self.SQb = self.YVb


class PoolSub(MixBase):
    def __init__(self, c, L, layer):
        self.common(c, layer)
        T = c.T; R = c.R
        W0 = c.A_W
        self.win = load_slots(c, L['m_in'], W0, 8192, lambda v: v.rearrange("p (k n) -> p k n", n=512), 'pwi')
        self.wgrp = load_slots(c, L['m_grp'], W0 + 16384, 1024, lambda v: v.rearrange("p (k n) -> p k n", n=256), 'pwg')
        self.wout = load_slots(c, L['m_out'], W0 + 20480, 2048, lambda v: v.rearrange("p (k n) -> p k n", n=128), 'pwo')
        E = 16 + T
        self.E = E
        base = W0 + 40960
        self.U = []; self.Ub = []; self.TA = []; self.TAb = []; self.TB = []; self.TBb = []
        for k in range(8):
            for lst, lstb, o in ((self.U, self.Ub, 0), (self.TA, self.TAb, 1), (self.TB, self.TBb, 2)):
                off = base + (o * 8 + k) * 4 * E
                lst.append(c.vf(off, E))
                lstb.append(R.alloc(off, 4 * E, f'pool{o}_{k}'))
        self.PB = c.vb(c.A_BS + 16 * T, 8 * T).rearrange("p (c t) -> p c t", t=T)
        self.PBb = [R.alloc(c.A_BS + 16 * T + 2 * T * k, 2 * T, f'PB{k}') for k in range(8)]
        for k in range(8):
            MEMSET(c, self.U[k][:, 0:16], 0.0, [self.Ub[k]])

    def tile(self, ti):
        c = self.c; T = c.T; E = self.E
        prenorm(c, self.gpre, self.SQ, self.SQb, self.rstd, self.rstdb)
        for m in range(8):
            U = self.U[m]; Ub = self.Ub[m]
            import os
            if ti > 0 and not os.environ.get('NOHALO'):
                CP(c, U[:, 0:16], U[:, T:T + 16], [Ub], [Ub])
            wv, wb = self.win[m // 4]
            ps, psb = c.next_ps()
            for k in range(8):
                MM(c, ps[:, 0:T], wv[:, k, (m % 4) * 128:(m % 4 + 1) * 128], c.XN[:, k, :], k == 0, k == 7, [wb, c.XNb], [psb])
            ACT(c, U[:, 16:E], ps[:, 0:T], AF.Copy, [psb], [Ub])
            g = m // 2
            TA = self.TA[m]; TAb = self.TAb[m]; TB = self.TB[m]; TBb = self.TBb[m]
            TT(c, TA[:, 1:E], U[:, 1:E], U[:, 0:E - 1], ALU.add, [Ub], [TAb])
            Wv, Wb = TA, TAb
            if g >= 1:
                TT(c, TB[:, 3:E], TA[:, 3:E], TA[:, 1:E - 2], ALU.add, [TAb], [TBb])
                Wv, Wb = TB, TBb
            if g >= 2:
                TT(c, TA[:, 7:E], TB[:, 7:E], TB[:, 3:E - 4], ALU.add, [TBb], [TAb])
                Wv, Wb = TA, TAb
            if g >= 3:
                TT(c, TB[:, 15:E], TA[:, 15:E], TA[:, 7:E - 8], ALU.add, [TAb], [TBb])
                Wv, Wb = TB, TBb
            w = 2 ** (g + 1)
            lo = 0
            if ti == 0:
                pinv = c.cst[:, C_PINV + g * 16: C_PINV + (g + 1) * 16]
                TT(c, Wv[:, 16:32], Wv[:, 16:32], pinv, ALU.mult, [Wb, c.cstb], [Wb])
                TT(c, self.PB[:, m, 0:16], Wv[:, 16:32], U[:, 16:32], ALU.subtract, [Wb, Ub], [self.PBb[m]])
                lo = 16
            STT(c, self.PB[:, m, lo:T], Wv[:, 16 + lo:E], 1.0 / w, U[:, 16 + lo:E], ALU.mult, ALU.subtract, [Wb, Ub], [self.PBb[m]])
        for g in range(4):
            wv, wb = self.wgrp[g]
            for oc in range(2):
                mo = 2 * g + oc
                ps, psb = c.next_ps()
                for k2 in range(2):
                    MM(c, ps[:, 0:T], wv[:, k2, oc * 128:(oc + 1) * 128], self.PB[:, 2 * g + k2, :], k2 == 0, k2 == 1,
                       [wb, self.PBb[2 * g + k2]], [psb])
                TS(c, self.YV[:, mo, :], ps[:, 0:T], c.cst[:, C_PSCALE + mo:C_PSCALE + mo + 1], None, ALU.mult, None, [psb, c.cstb], [self.YVb[mo]])
        out_proj_residual(c, self.wout, self.YV, self.YVb, self.gpost, self.Y, self.Yflat, self.Yb, self.rstd, self.rstdb)


class HgrnSub(MixBase):
    def __init__(self, c, L, layer):
        self.common(c, layer)
        T = c.T; R = c.R
        self.layer = layer
        self.j = layer // 3
        W0 = c.A_W
        self.win = load_slots(c, L['m_in'], W0, 8192, lambda v: v.rearrange("p (k n) -> p k n", n=512), 'hwi')
        self.wout = load_slots(c, L['m_out'], W0 + 65536, 2048, lambda v: v.rearrange("p (k n) -> p k n", n=128), 'hwo')
        o = W0 + 81920
        self.St = []; self.Sb = []
        for hd in range(8):
            self.St.append(c.vf(o, 128)); self.Sb.append(R.alloc(o, 512, f'S{hd}')); o += 512
            MEMSET(c, self.St[hd], 0.0, [self.Sb[hd]])
        self.lb = c.vf(o, 8); self.oml = c.vf(o + 32, 8); self.lbE = c.vf(o + 64, 32); self.lbden = c.vf(o + 192, 8)
        self.lbb = R.alloc(o, 256, 'lb'); o += 256
        self.smask = c.vf(o, T); self.smb = R.alloc(o, 4 * T, 'smask'); o += 4 * T
        MEMSET(c, self.smask, 1.0, [self.smb])
        MEMSET(c, self.smask.rearrange("p (b l) -> p b l", l=16)[:, :, 0:1], 0.0, [self.smb])
        raw = c.cst[:, C_LBRAW:C_LBRAW + 32]
        if layer == 0:
            MEMSET(c, self.lb, 0.0, [self.lbb])
        else:
            ACT(c, self.lbE, raw, AF.Exp, [c.cstb], [self.lbb])
            E3 = self.lbE.rearrange("p (c d) -> p c d", d=4)
            c.P.op('dve', lambda e: e.tensor_reduce(out=self.lbden, in_=E3, axis=AX.X, op=ALU.add), [self.lbb], [self.lbb])
            c.P.op('dve', lambda e: e.tensor_reduce(out=self.lb, in_=E3[:, :, 1:layer + 1], axis=AX.X, op=ALU.add), [self.lbb], [self.lbb])
            RECIP(c, self.lbden, self.lbden, [self.lbb], [self.lbb])
            TT(c, self.lb, self.lb, self.lbden, ALU.mult, [self.lbb], [self.lbb])
        TS(c, self.oml, self.lb, -1.0, 1.0, ALU.mult, ALU.add, [self.lbb], [self.lbb])
        G = T // 128
        self.G = G
        self.Vt = c.vb(o, G * 1024).rearrange("p (g n) -> p g n", n=1024)
        self.Vtb = [R.alloc(o + g * 2048, 2048, f'Vt{g}') for g in range(G)]
        o += G * 2048
        free = [[o, c.A_W + c.WBYTES], [c.A_BS + 16 * T, c.A_BS + 44 * T], [c.A_FS + 36 * T, c.A_FS + 48 * T]]

        def take(nbytes):
            for fr in free:
                if fr[1] - fr[0] >= nbytes:
                    a = fr[0]
                    fr[0] += nbytes
                    return a
            raise AssertionError(("hgrn sbuf overflow", nbytes, free))
        self.sets = []
        for par in range(2):
            st = {}
            for nm in ('Q', 'F', 'K', 'B', 'D', 'E', 'QI', 'KS', 'O', 'Gt'):
                a = take(4 * T)
                st[nm] = c.vf(a, T); st[nm + 'b'] = R.alloc(a, 4 * T, f'{nm}{par}')
            for nm in ('QR', 'KR', 'SQh'):
                a = take(2 * T)
                st[nm] = c.vb(a, T); st[nm + 'b'] = R.alloc(a, 2 * T, f'{nm}{par}')
            a = take(max(T // 4, 32))
            st['DEC'] = c.vf(a, T // 16); st['DECb'] = R.alloc(a, max(T // 4, 32), f'DEC{par}')
            a = take(256)
            st['AT'] = c.vb(a, 128); st['ATb'] = R.alloc(a, 256, f'AT{par}')
            a = take(256)
            st['KSt'] = c.vb(a, 128); st['KStb'] = R.alloc(a, 256, f'KSt{par}')
            a = take(2048)
            st['KSm'] = c.vb(a, 1024).rearrange("p (j n) -> p j n", n=128)
            st['KSmb'] = [R.alloc(a + j * 256, 256, f'KSm{par}_{j}') for j in range(8)]
            self.sets.append(st)
        self.gi = 0
        c.ps_lim = 6

    def nq(self):
        ps, psb = self.c.next_ps()
        return ps[:, 0:128], psb

    def tile(self, ti):
        c = self.c; T = c.T; G = self.G
        c.ps_lim = 6
        prenorm(c, self.gpre, self.SQ, self.SQb, self.rstd, self.rstdb)
        for g in range(G):
            for half in range(2):
                wv, wb = self.win[4 + half]
                ps, psb = c.next_ps()
                for k in range(8):
                    MM(c, ps[:, 0:512], c.XN[:, k, g * 128:(g + 1) * 128], wv[:, k, :], k == 0, k == 7, [wb, c.XNb], [psb])
                ACT(c, self.Vt[:, g, half * 512:(half + 1) * 512], ps[:, 0:512], AF.Copy, [psb], [self.Vtb[g]])
        gn = c.cst[:, C_GNORM + self.j:C_GNORM + self.j + 1]
        for hd in range(8):
            st = self.sets[hd % 2]
            hs = (hd % 4) * 128
            Sv = self.St[hd]; Sb = self.Sb[hd]

            def proj(slot, func, out, outb):
                wv, wb = self.win[slot]
                ps, psb = c.next_ps()
                for k in range(8):
                    MM(c, ps[:, 0:T], wv[:, k, hs:hs + 128], c.XN[:, k, :], k == 0, k == 7, [wb, c.XNb], [psb])
                ACT(c, out, ps[:, 0:T], func, [psb], [outb])
            proj(hd // 4, AF.Copy, st['Q'], st['Qb'])
            proj(2 + hd // 4, AF.Sigmoid, st['F'], st['Fb'])
            proj(6 + hd // 4, AF.Silu, st['Gt'], st['Gtb'])
            TS(c, st['F'], st['F'], self.oml[:, hd:hd + 1], self.lb[:, hd:hd + 1], ALU.mult, ALU.add, [st['Fb'], self.lbb], [st['Fb']])
            ACT(c, st['E'], st['F'], AF.Ln, [st['Fb']], [st['Eb']])
            TS(c, st['K'], st['F'], -1.0, 1.0, ALU.mult, ALU.add, [st['Fb']], [st['Kb']])
            c.P.op('dve', lambda e, st=st: e.tensor_tensor_scan(out=st['B'], data0=self.smask, data1=st['E'], initial=0.0,
                                                                 op0=ALU.mult, op1=ALU.add),
                   [st['Eb'], self.smb], [st['Bb']])
            NB = T // 16
            B3 = st['B'].rearrange("p (b l) -> p b l", l=16)
            D3 = st['D'].rearrange("p (b l) -> p b l", l=16)
            bmid = B3[:, :, 8:9].broadcast_to([128, NB, 16])
            blast = B3[:, :, 15:16].broadcast_to([128, NB, 16])
            TT(c, D3, B3, bmid, ALU.subtract, [st['Bb']], [st['Db']])
            ACT(c, st['E'], st['D'], AF.Exp, [st['Db']], [st['Eb']])
            TT(c, st['QR'], st['Q'], st['E'], ALU.mult, [st['Qb'], st['Eb']], [st['QRb']])
            ACT(c, st['E'], st['D'], AF.Exp, [st['Db']], [st['Eb']], scale=-1.0)
            TT(c, st['KR'], st['K'], st['E'], ALU.mult, [st['Kb'], st['Eb']], [st['KRb']])
            ACT(c, st['E'], st['B'], AF.Exp, [st['Bb']], [st['Eb']])
            TT(c, st['QI'], st['Q'], st['E'], ALU.mult, [st['Qb'], st['Eb']], [st['QIb']])
            TT(c, D3, blast, B3, ALU.subtract, [st['Bb']], [st['Db']])
            ACT(c, st['E'], st['D'], AF.Exp, [st['Db']], [st['Eb']])
            TT(c, st['KS'], st['K'], st['E'], ALU.mult, [st['Kb'], st['Eb']], [st['KSb']])
            ACT(c, st['DEC'], st['B'].rearrange("p (b l) -> p b l", l=16)[:, :, 15], AF.Exp, [st['Bb']], [st['DECb']])
            for g in range(G):
                gs = slice(g * 128, (g + 1) * 128)
                vh = self.Vt[:, g, hd * 128:(hd + 1) * 128]
                pa, pab = self.nq()
                MM(c, pa, st['KR'][:, gs], st['QR'][:, gs], True, True, [st['KRb'], st['QRb']], [pab])
                TT(c, st['AT'], pa, c.cst[:, C_HMASK:C_HMASK + 128], ALU.mult, [pab, c.cstb], [st['ATb']])
                po, pob = self.nq()
                MM(c, po, vh, st['AT'], True, True, [self.Vtb[g], st['ATb']], [pob])
                ACT(c, st['O'][:, gs], po, AF.Copy, [pob], [st['Ob']])
                pt, ptb = self.nq()
                TR(c, pt, st['KS'][:, gs], c.identf, [st['KSb'], c.cstb], [ptb])
                for j in range(8):
                    ACT(c, st['KSm'][:, j, :], pt, AF.Copy, [ptb, c.cstb], [st['KSmb'][j]], scale=c.cst[:, C_BLKM + j:C_BLKM + j + 1])
                bk = 6 + (self.gi % 2)
                self.gi += 1
                pi, pib = c.ps[bk][:, 0:128], c.psb[bk]
                for j in range(8):
                    blk = g * 8 + j
                    MM(c, pi[:, j * 16:(j + 1) * 16], Sv, st['QI'][:, blk * 16:(blk + 1) * 16], True, True, [Sb, st['QIb']], [pib])
                    pu, pub = self.nq()
                    MM(c, pu, st['KSm'][:, j, :], vh, True, True, [st['KSmb'][j], self.Vtb[g]], [pub])
                    STT(c, Sv, Sv, st['DEC'][:, blk:blk + 1], pu, ALU.mult, ALU.add, [Sb, st['DECb'], pub], [Sb])
                TT(c, st['O'][:, gs], st['O'][:, gs], pi, ALU.add, [pib, st['Ob']], [st['Ob']])
            ACT(c, st['SQh'], st['O'], AF.Square, [st['Ob']], [st['SQhb']])
            ps, psb = c.next_ps()
            MM(c, ps[:, 0:T], c.ones_h, st['SQh'], True, True, [st['SQhb'], c.miscb], [psb])
            rstd_from_ps(c, ps, psb, st['D'], st['Db'])
            STT(c, st['O'], st['O'], gn, st['D'], ALU.mult, ALU.mult, [st['Ob'], st['Db'], c.cstb], [st['Ob']])
            TT(c, self.YV[:, hd, :], st['O'], st['Gt'], ALU.mult, [st['Ob'], st['Gtb']], [self.YVb[hd]])
        out_proj_residual(c, self.wout, self.YV, self.YVb, self.gpost, self.Y, self.Yflat, self.Yb, self.rstd, self.rstdb)


def extra_inputs(S, T):
    NR = T // 128
    tab = np.zeros((128, (NR + 1) * T), np.float32)
    kk = np.arange(128)[:, None].astype(np.float64)
    qq = np.arange(T)[None, :].astype(np.float64)
    tab[:, 0:T] = kk - qq
    for r in range(NR):
        kpos = 128 * r + kk
        allowed = (kpos // 64) <= (qq // 64)
        tab[:, (r + 1) * T:(r + 2) * T] = np.where(allowed, -np.abs(qq - kpos), -1.0e6)
    return {"atab": tab}


class AttnSub(MixBase):
    def __init__(self, c, L, layer):
        self.common(c, layer)
        T = c.T; R = c.R; S = c.S
        self.layer = layer
        W0 = c.A_W
        self.win = load_slots(c, L['m_in'], W0, 8192, lambda v: v.rearrange("p (k n) -> p k n", n=512), 'awi')
        self.wout = load_slots(c, L['m_out'], W0 + 49152, 2048, lambda v: v.rearrange("p (k n) -> p k n", n=128), 'awo')
        o = W0 + 65536
        self.Kb = []; self.Kbb = []; self.Vb = []; self.Vbb = []; self.Kst = []; self.Kstb = []
        for i in range(2):
            self.Kb.append(c.vb(o, S)); self.Kbb.append(R.alloc(o, 2 * S, f'Kb{i}')); o += 2 * S
            self.Vb.append(c.vb(o, S).rearrange("p (t n) -> p t n", n=128)); self.Vbb.append(R.alloc(o, 2 * S, f'Vb{i}')); o += 2 * S
        for i in range(2):
            self.Kst.append(c.vb(o, T)); self.Kstb.append(R.alloc(o, 2 * T, f'Kst{i}')); o += 2 * T
        self.sm = c.vf(o, 80); self.smb = R.alloc(o, 320, 'attn_small'); o += 320
        assert o <= c.A_W + c.WBYTES, o - c.A_W
        lam_init = 0.8 - 0.6 * math.exp(-0.3 * layer)
        lamv = c.cst[:, C_LAM:C_LAM + 256]
        pr = self.sm[:, 0:64]; s1 = self.sm[:, 64:65]; s2 = self.sm[:, 65:66]
        self.nlam = self.sm[:, 66:67]; self.subl = self.sm[:, 67:68]
        TT(c, pr, lamv[:, 0:64], lamv[:, 64:128], ALU.mult, [c.cstb], [self.smb])
        c.P.op('dve', lambda e: e.tensor_reduce(out=s1, in_=pr, axis=AX.X, op=ALU.add), [self.smb], [self.smb])
        TT(c, pr, lamv[:, 128:192], lamv[:, 192:256], ALU.mult, [c.cstb, self.smb], [self.smb])
        c.P.op('dve', lambda e: e.tensor_reduce(out=s2, in_=pr, axis=AX.X, op=ALU.add), [self.smb], [self.smb])
        ACT(c, s1, s1, AF.Exp, [self.smb], [self.smb])
        ACT(c, s2, s2, AF.Exp, [self.smb], [self.smb])
        TT(c, s1, s2, s1, ALU.subtract, [self.smb], [self.smb])
        TS(c, self.nlam, s1, -lam_init, None, ALU.add, None, [self.smb], [self.smb])
        TS(c, self.subl, c.cst[:, C_SUBLN:C_SUBLN + 1], 1.0 - lam_init, None, ALU.mult, None, [c.cstb, self.smb], [self.smb])
        NR = T // 128
        self.NR = NR
        ob = c.A_BS + 16 * T
        self.tab = c.vf(ob, (NR + 1) * T)
        self.tabb = R.alloc(ob, 4 * T * (NR + 1), 'atab')
        DMA(c, 'sp', self.tab, c.atab[:, :], [], [self.tabb])
        ob += 4 * T * (NR + 1)
        self.tmp = []; self.tmpb = []
        for i in range(2):
            self.tmp.append(c.vf(ob, T)); self.tmpb.append(R.alloc(ob, 4 * T, f'atmp{i}')); ob += 4 * T
        assert ob <= c.A_BS + 44 * T, (ob - c.A_BS, 44 * T)
        of = c.A_FS + 36 * T
        self.Qb = []; self.Qbb = []; self.PT = []; self.PTb = []
        for i in range(2):
            self.Qb.append(c.vb(of, T)); self.Qbb.append(R.alloc(of, 2 * T, f'Qb{i}')); of += 2 * T
        for i in range(4):
            self.PT.append(c.vb(of, T)); self.PTb.append(R.alloc(of, 2 * T, f'PT{i}')); of += 2 * T
        assert of <= c.A_FS + 48 * T
        self.ones_k = c.vb(c.A_MISC + 1024, 128)
        MEMSET(c, self.ones_k, 1.0, [c.miscb])
        self.Kd = [Buf(f'ktd{h}') for h in range(8)]
        self.Vd = [Buf(f'vd{h}') for h in range(8)]
        self.pi = 0

    def tile(self, ti):
        c = self.c; T = c.T; S = c.S; NR = self.NR
        G = T // 128
        t0 = ti * T
        c.ps_base = 4; c.ps_lim = 4
        prenorm(c, self.gpre, self.SQ, self.SQb, self.rstd, self.rstdb)
        for hd in range(8):
            wv, wb = self.win[2 + hd // 4]
            hs = (hd % 4) * 128
            ps, psb = c.next_ps()
            for k in range(8):
                MM(c, ps[:, 0:T], wv[:, k, hs:hs + 128], c.XN[:, k, :], k == 0, k == 7, [wb, c.XNb], [psb])
            ks = self.Kst[hd % 2]; ksb = self.Kstb[hd % 2]
            ACT(c, ks, ps[:, 0:T], AF.Copy, [psb], [ksb])
            DMA(c, 'sp', c.ktd[hd, :, t0:t0 + T], ks, [ksb], [self.Kd[hd]])
        Vst = c.vb(c.A_BS, 8 * T).rearrange("p (g n) -> p g n", n=1024)
        for g in range(G):
            for half in range(2):
                wv, wb = self.win[4 + half]
                ps, psb = c.next_ps()
                for k in range(8):
                    MM(c, ps[:, 0:512], c.XN[:, k, g * 128:(g + 1) * 128], wv[:, k, :], k == 0, k == 7, [wb, c.XNb], [psb])
                ACT(c, Vst[:, g, half * 512:(half + 1) * 512], ps[:, 0:512], AF.Copy, [psb], self.YVb)
        for hd in range(8):
            DMA(c, 'sp', c.vd[hd, :, ti * G:(ti + 1) * G, :], Vst[:, :, hd * 128:(hd + 1) * 128], self.YVb, [self.Vd[hd]])
        nk = (ti + 1) * T
        nkt = nk // 128
        scale = 0.125
        for hd in range(8):
            par = hd % 2
            slope = 2.0 ** (-(hd + 1))
            Kb = self.Kb[par]; Kbb = self.Kbb[par]; Vb = self.Vb[par]; Vbb = self.Vbb[par]
            DMA(c, 'sp', Kb[:, 0:nk], c.ktd[hd, :, 0:nk], [self.Kd[hd]], [Kbb])
            DMA(c, 'sp', Vb[:, 0:nkt, :], c.vd[hd, :, 0:nkt, :], [self.Vd[hd]], [Vbb])
            wv, wb = self.win[hd // 4]
            hs = (hd % 4) * 128
            ps, psb = c.next_ps()
            for k in range(8):
                MM(c, ps[:, 0:T], wv[:, k, hs:hs + 128], c.XN[:, k, :], k == 0, k == 7, [wb, c.XNb], [psb])
            Qb = self.Qb[par]; Qbb = self.Qbb[par]
            ACT(c, Qb, ps[:, 0:T], AF.Copy, [psb], [Qbb])
            for g2 in range(2):
                oacc, oaccb = c.ps[2 * g2], c.psb[2 * g2]
                dacc, daccb = c.ps[2 * g2 + 1], c.psb[2 * g2 + 1]
                rows = slice(g2 * 64, (g2 + 1) * 64)
                blocks = []
                for kt in range(nkt):
                    if kt < ti * G:
                        blocks.append((kt, 0, self.tab[:, 0:T], float(t0 - 128 * kt)))
                    else:
                        r = kt - ti * G
                        blocks.append((kt, 128 * r, self.tab[:, (r + 1) * T:(r + 2) * T], 0.0))
                LA = 3
                pend = []
                for n in range(len(blocks) + LA):
                    if n < len(blocks):
                        kt, c0, tb, delta = blocks[n]
                        ps, psb = c.next_ps()
                        MM(c, ps[:, c0:T], Kb[rows, kt * 128:(kt + 1) * 128], Qb[rows, c0:T], True, True, [Kbb, Qbb], [psb])
                        i = self.pi % 4
                        self.pi += 1
                        PT = self.PT[i]; PTb = self.PTb[i]
                        STT(c, ps[:, c0:T], ps[:, c0:T], scale / slope, tb[:, c0:T], ALU.mult, ALU.add, [psb, self.tabb], [psb])
                        ACT(c, PT[:, c0:T], ps[:, c0:T], AF.Exp, [psb], [PTb], bias=float(-slope * delta), scale=slope)
                        pend.append((kt, c0, PT, PTb))
                    if n >= LA:
                        kt, c0, PT, PTb = pend[n - LA]
                        MM(c, oacc[:, c0:T], Vb[:, kt, :], PT[:, c0:T], kt == 0, kt == nkt - 1, [Vbb, PTb], [oaccb])
                        MM(c, dacc[:, c0:T], self.ones_k, PT[:, c0:T], kt == 0, kt == nkt - 1, [c.miscb, PTb], [daccb])
            t0v, t0b, t1v, t1b = self.tmp[0], self.tmpb[0], self.tmp[1], self.tmpb[1]
            RECIP(c, t0v, c.ps[1][:, 0:T], [c.psb[1]], [t0b])
            TT(c, t0v, c.ps[0][:, 0:T], t0v, ALU.mult, [c.psb[0], t0b], [t0b])
            RECIP(c, t1v, c.ps[3][:, 0:T], [c.psb[3]], [t1b])
            TT(c, t1v, c.ps[2][:, 0:T], t1v, ALU.mult, [c.psb[2], t1b], [t1b])
            STT(c, t0v, t1v, self.nlam, t0v, ALU.mult, ALU.add, [t0b, t1b, self.smb], [t0b])
            sq = self.PT[0]; sqb = self.PTb[0]
            ACT(c, sq, t0v, AF.Square, [t0b], [sqb])
            ps, psb = c.next_ps()
            MM(c, ps[:, 0:T], c.ones_h, sq, True, True, [sqb, c.miscb], [psb])
            rstd_from_ps(c, ps, psb, t1v, t1b)
            STT(c, self.YV[:, hd, :], t0v, self.subl, t1v, ALU.mult, ALU.mult, [t0b, t1b, self.smb], [self.YVb[hd]])
        c.ps_base = 0; c.ps_lim = 8
        out_proj_residual(c, self.wout, self.YV, self.YVb, self.gpost, self.Y, self.Yflat, self.Yb, self.rstd, self.rstdb)

    def biasc(self, v):
        return float(v)


SEQ = 8192
TILE = 512
ALL_SUBLAYERS = [(l, k) for l in range(DEPTH) for k in ('f1', 'mix', 'f2')]


def kernel(x, norm_gains, ffn1_w_in, ffn1_w_out, ffn2_w_in, ffn2_w_out,
           hgrn_w_in, hgrn_gnorm, hgrn_w_out, hgrn_lb_raw,
           diff_w_in, diff_lambda, diff_subln, diff_w_out,
           pool_w_in, pool_w_group, pool_scale, pool_w_out):
    inp = dict(x=x, norm_gains=norm_gains, ffn1_w_in=ffn1_w_in, ffn1_w_out=ffn1_w_out,
               ffn2_w_in=ffn2_w_in, ffn2_w_out=ffn2_w_out, hgrn_w_in=hgrn_w_in, hgrn_gnorm=hgrn_gnorm,
               hgrn_w_out=hgrn_w_out, hgrn_lb_raw=hgrn_lb_raw, diff_w_in=diff_w_in, diff_lambda=diff_lambda,
               diff_subln=diff_subln, diff_w_out=diff_w_out, pool_w_in=pool_w_in, pool_w_group=pool_w_group,
               pool_scale=pool_scale, pool_w_out=pool_w_out)
    inp = {k: np.asarray(v, dtype=np.float32) for k, v in inp.items()}
    B, S, _ = inp['x'].shape
    assert S == SEQ and B == 8
    ws, plan = plan_weights(inp)
    wst = ws.array()
    cst = pack_consts(inp)
    ext = extra_inputs(S, TILE)
    nc = build(S, TILE, plan, wst.shape[1], ALL_SUBLAYERS)
    in_maps = []
    for b in range(B):
        in_maps.append({"xT": np.ascontiguousarray(inp['x'][b].T), "wst": wst, "cst": cst, "atab": ext["atab"]})
    res = run_bass_kernel_spmd(nc, in_maps, core_ids=list(range(B)))
    out = np.empty((B, S, D), np.float32)
    for b in range(B):
        out[b] = res.results[b]["oT"].T
    return out
```

```python
import math
import numpy as np
import concourse.bass as bass
import concourse.mybir as mybir
from concourse.bass_utils import run_bass_kernel_spmd

F32 = mybir.dt.float32
BF16 = mybir.dt.bfloat16
AF = mybir.ActivationFunctionType
ALU = mybir.AluOpType
AX = mybir.AxisListType

D = 1024
NC8 = 8
DFF = 2816
NJ = 22
DEPTH = 4
EPS = 1e-6
ENGS = ['pe', 'act', 'dve', 'pool', 'sp']
SAME_SYNC = {'pe': False, 'act': True, 'dve': True, 'pool': True, 'sp': True}
EPOCH = 30000
NDSEM = 16


class Op:
    __slots__ = ('eng', 'fn', 'deps', 'dma', 'dsem', 'dval', 'signal', 'count', 'idx')


class Buf:
    __slots__ = ('name', 'w', 'r', 'lo', 'hi')

    def __init__(self, name=''):
        self.name = name
        self.w = {}
        self.r = {}


def _key(o):
    return ('d', o.dsem) if o.dma else o.eng


def _ord(o):
    return o.dval if o.dma else o.idx


class Prog:
    def __init__(self, nc):
        self.nc = nc
        self.ops = {e: [] for e in ENGS}
        self.dsem_val = [0] * NDSEM
        self.dsem_last = [None] * NDSEM
        self.next_dsem = {'sp': 0, 'pool': 0, 'act': 0}

    def op(self, eng, fn, reads=(), writes=(), dma=False):
        o = Op()
        o.eng = eng; o.fn = fn; o.dma = dma; o.signal = False; o.count = None
        o.idx = len(self.ops[eng]); o.dsem = None; o.dval = None
        deps = {}

        def add(d):
            k = _key(d)
            c = deps.get(k)
            if c is None or _ord(d) > _ord(c):
                deps[k] = d
        for b in reads:
            for d in b.w.values():
                add(d)
        for b in writes:
            for d in b.w.values():
                add(d)
            for d in b.r.values():
                add(d)
        if dma:
            half = NDSEM // 2
            base = 0 if eng == 'sp' else half
            s = base + self.next_dsem[eng]
            self.next_dsem[eng] = (self.next_dsem[eng] + 1) % half
            if self.dsem_last[s] is not None:
                add(self.dsem_last[s])
            o.dsem = s
            self.dsem_val[s] += 16
            o.dval = self.dsem_val[s]
            self.dsem_last[s] = o
        o.deps = [d for d in deps.values() if d.dma or d.eng != eng or SAME_SYNC[eng]]
        for d in o.deps:
            if not d.dma:
                d.signal = True
        k = _key(o)
        for b in reads:
            b.r[k] = o
        for b in writes:
            b.w = {k: o}
            b.r = {}
        self.ops[eng].append(o)
        return o

    def emit(self):
        nc = self.nc
        nsig = {}
        for e in ENGS:
            c = 0
            for o in self.ops[e]:
                if o.signal:
                    c += 1
                    o.count = c
            nsig[e] = c
        esems = {e: [nc.alloc_semaphore(f"s_{e}_{i}") for i in range(max(1, (nsig[e] + EPOCH - 1) // EPOCH))]
                 for e in ENGS}
        dsems = [nc.alloc_semaphore(f"s_dma_{i}") for i in range(NDSEM)]

        def semval(d):
            if d.dma:
                return ('d', d.dsem), dsems[d.dsem], d.dval
            ep = (d.count - 1) // EPOCH
            return (d.eng, ep), esems[d.eng][ep], d.count - ep * EPOCH

        ops = self.ops
        dsem_val = self.dsem_val

        def run(eng, e):
            waited = {}
            for o in ops[eng]:
                for d in o.deps:
                    k, s, v = semval(d)
                    if waited.get(k, 0) >= v:
                        continue
                    waited[k] = v
                    e.wait_ge(s, v)
                ins = o.fn(e)
                if o.dma:
                    ins.then_inc(dsems[o.dsem], 16)
                elif o.signal:
                    _, s, _ = semval(o)
                    ins.then_inc(s, 1)
            if eng == 'sp':
                for i in range(NDSEM):
                    if dsem_val[i] > 0:
                        e.wait_ge(dsems[i], dsem_val[i])

        with nc.Block() as block:
            @block.tensor
            def _(e):
                run('pe', e)

            @block.scalar
            def _(e):
                run('act', e)

            @block.vector
            def _(e):
                run('dve', e)

            @block.gpsimd
            def _(e):
                run('pool', e)

            @block.sync
            def _(e):
                run('sp', e)


class Region:
    def __init__(self):
        self.bufs = []

    def alloc(self, lo, n, name=''):
        b = Buf(name)
        b.lo = lo; b.hi = lo + n
        for o in self.bufs:
            if o.lo < b.hi and b.lo < o.hi:
                for k, d in o.w.items():
                    c = b.w.get(k)
                    if c is None or _ord(d) > _ord(c):
                        b.w[k] = d
                for k, d in o.r.items():
                    c = b.r.get(k)
                    if c is None or _ord(d) > _ord(c):
                        b.r[k] = d
        self.bufs.append(b)
        return b


def pack_cols(W, col_groups):
    K = W.shape[0]
    KC = K // 128
    Wr = W.reshape(KC, 128, W.shape[1])
    outs = []
    for cols in col_groups:
        blk = Wr[:, :, cols]
        outs.append(np.ascontiguousarray(blk.transpose(1, 0, 2)).reshape(128, -1))
    return outs


class WStream:
    def __init__(self):
        self.parts = []
        self.off = 0

    def add(self, arr):
        o = self.off
        self.parts.append(arr)
        self.off += arr.shape[1]
        return (o, arr.shape[1])

    def array(self):
        return np.ascontiguousarray(np.concatenate(self.parts, axis=1).astype(np.float32))


def ffn_groups():
    gs = []
    for s in range(11):
        c0 = s * 256
        gs.append(np.concatenate([np.arange(c0, c0 + 256), DFF + np.arange(c0, c0 + 256)]))
    return gs


def out_groups(n_out=D):
    return [np.arange(m * 128, (m + 1) * 128) for m in range(n_out // 128)]


def plan_weights(inputs):
    ws = WStream()
    plan = []
    for i in range(DEPTH):
        kind, j = i % 3, i // 3
        L = {}
        for nm, win, wout in (('f1', 'ffn1_w_in', 'ffn1_w_out'), ('f2', 'ffn2_w_in', 'ffn2_w_out')):
            L[nm + '_in'] = [ws.add(a) for a in pack_cols(inputs[win][i], ffn_groups())]
            L[nm + '_out'] = [ws.add(a) for a in pack_cols(inputs[wout][i], out_groups())]
        if kind == 0:
            W = inputs['hgrn_w_in'][j]
            L['m_in'] = [ws.add(a) for a in pack_cols(W, [np.arange(c * 512, (c + 1) * 512) for c in range(8)])]
            L['m_out'] = [ws.add(a) for a in pack_cols(inputs['hgrn_w_out'][j], out_groups())]
        elif kind == 1:
            W = inputs['diff_w_in'][j]
            L['m_in'] = [ws.add(a) for a in pack_cols(W, [np.arange(c * 512, (c + 1) * 512) for c in range(6)])]
            L['m_out'] = [ws.add(a) for a in pack_cols(inputs['diff_w_out'][j], out_groups())]
        else:
            L['m_in'] = [ws.add(a) for a in pack_cols(inputs['pool_w_in'][j], [np.arange(c * 512, (c + 1) * 512) for c in range(2)])]
            wg = inputs['pool_w_group'][j]
            L['m_grp'] = [ws.add(pack_cols(wg[g], [np.arange(256)])[0]) for g in range(4)]
            L['m_out'] = [ws.add(a) for a in pack_cols(inputs['pool_w_out'][j], out_groups())]
        plan.append(L)
    return ws, plan


C_GAIN = 0
C_GNORM = 192
C_SUBLN = 194
C_PSCALE = 195
C_LBRAW = 203
C_LAM = 235
C_IDENT = 491
C_HMASK = 619
C_PINV = 747
C_BLKM = 811
NCST = 819


def pack_consts(inp):
    c = np.zeros((128, NCST), np.float32)
    c[:, C_GAIN:C_GAIN + 192] = inp['norm_gains'].reshape(4, 6, 8, 128).transpose(3, 0, 1, 2).reshape(128, 192)
    c[:, C_GNORM:C_GNORM + 2] = inp['hgrn_gnorm'].T
    c[:, C_SUBLN:C_SUBLN + 1] = inp['diff_subln'].T
    c[:, C_PSCALE:C_PSCALE + 8] = inp['pool_scale'][0].reshape(8, 128).T
    c[:, C_LBRAW:C_LBRAW + 32] = inp['hgrn_lb_raw'].reshape(4, 8, 128).transpose(2, 1, 0).reshape(128, 32)
    c[:, C_LAM:C_LAM + 256] = np.broadcast_to(inp['diff_lambda'][0].reshape(1, 256), (128, 256))
    c[:, C_IDENT:C_IDENT + 128] = np.eye(128, dtype=np.float32)
    s = np.arange(128)[:, None]; t = np.arange(128)[None, :]
    c[:, C_HMASK:C_HMASK + 128] = ((s // 16 == t // 16) & (s <= t)).astype(np.float32)
    pinv = np.zeros((4, 16), np.float32)
    for g, w in enumerate((2, 4, 8, 16)):
        pinv[g] = 1.0 / np.minimum(np.arange(16) + 1, w)
    c[:, C_PINV:C_PINV + 64] = np.broadcast_to(pinv.reshape(1, 64), (128, 64))
    c[:, C_BLKM:C_BLKM + 8] = (np.arange(128)[:, None] // 16 == np.arange(8)[None, :]).astype(np.float32)
    return c


class Ctx:
    pass


def build(S, T, plan, wtot, sublayers, first_src_is_x=True):
    nc = bass.Bass("TRN2", target_bir_lowering=False)
    P = Prog(nc)
    NT = S // T
    xT = nc.dram_tensor("xT", [D, S], F32, kind="ExternalInput").ap()
    wst = nc.dram_tensor("wst", [128, wtot], F32, kind="ExternalInput").ap()
    cstd = nc.dram_tensor("cst", [128, NCST], F32, kind="ExternalInput").ap()
    oT = nc.dram_tensor("oT", [D, S], F32, kind="ExternalOutput").ap()
    hscr = nc.dram_tensor("hscr", [D, S], F32).ap()
    NR = T // 128
    atab = nc.dram_tensor("atab", [128, (NR + 1) * T], F32, kind="ExternalInput").ap()
    ktd = nc.dram_tensor("ktd", [8, 128, S], BF16).ap()
    vd = nc.dram_tensor("vd", [8, 128, S // 128, 128], BF16).ap()

    A_CST = 0
    A_MISC = 3328
    A_H = A_MISC + 1280
    A_XN = A_H + 32 * T
    A_FS = A_XN + 16 * T
    A_BS = A_FS + 48 * T
    A_W = A_BS + 44 * T
    WBYTES = 135168
    A_END = A_W + WBYTES
    arena = nc.alloc_sbuf_tensor("arena", [128, A_END // 2], BF16)
    R = Region()

    def vb(off, n):
        return arena[:, off // 2: off // 2 + n]

    def vf(off, n):
        return arena[:, off // 2: off // 2 + 2 * n].bitcast(F32)

    c = Ctx()
    c.nc = nc; c.P = P; c.R = R; c.T = T; c.S = S; c.NT = NT
    c.vb = vb; c.vf = vf; c.wst = wst
    c.atab = atab; c.ktd = ktd; c.vd = vd
    c.A_MISC = A_MISC; c.A_W = A_W; c.A_FS = A_FS; c.A_BS = A_BS; c.A_XN = A_XN; c.A_H = A_H; c.WBYTES = WBYTES
    cst = vf(A_CST, NCST)
    c.cst = cst
    cstb = R.alloc(A_CST, NCST * 4, 'cst')
    c.cstb = cstb
    c.P = P
    DMA(c, 'sp', cst, cstd[:, :], [], [cstb])
    ones_d = vb(A_MISC, 128); ones_h = vb(A_MISC + 256, 128); identb = vb(A_MISC + 512, 128)
    miscb = R.alloc(A_MISC, 1280, "misc")
    c.ones_d = ones_d; c.ones_h = ones_h; c.identb = identb; c.miscb = miscb
    c.identf = cst[:, C_IDENT:C_IDENT + 128]
    c.epsc = vf(A_MISC + 768, 1)
    MEMSET(c, c.epsc, EPS, [miscb])
    MEMSET(c, ones_d, 1.0 / D, [miscb])
    MEMSET(c, ones_h, 1.0 / 128, [miscb])
    CP(c, identb, cst[:, C_IDENT:C_IDENT + 128], [cstb], [miscb])

    c.ps = [nc.alloc_psum_tensor(f"ps{i}", [128, 512], F32) for i in range(8)]
    c.PR = Region()
    c.psi = 0
    c.ps_lim = 8

    def reset_ps():
        c.psb = [c.PR.alloc(i * 2048, 2048, f"ps{i}") for i in range(8)]
        c.ps_lim = 8
        c.psi = 0
        c.ps_base = 0
    c.reset_ps = reset_ps
    reset_ps()

    c.ps_base = 0

    def next_ps():
        i = c.psi % c.ps_lim
        c.psi = (i + 1) % c.ps_lim
        return c.ps[c.ps_base + i], c.psb[c.ps_base + i]
    c.next_ps = next_ps

    c.H = vf(A_H, 8 * T).rearrange("p (c t) -> p c t", t=T)
    c.Hflat = vf(A_H, 8 * T)
    c.Hb = R.alloc(A_H, 32 * T, 'H')
    c.XN = vb(A_XN, 8 * T).rearrange("p (c t) -> p c t", t=T)
    c.XNflat = vb(A_XN, 8 * T)
    c.XNb = R.alloc(A_XN, 16 * T, 'XN')

    dt = {}

    def dtile(name, i):
        k = (name, i)
        if k not in dt:
            dt[k] = Buf(f"{name}{i}")
        return dt[k]

    tens = {'x': xT, 'h': hscr, 'o': oT}
    nsl = len(sublayers)
    c.state = {}
    for si, (layer, kind) in enumerate(sublayers):
        src = 'x' if si == 0 else 'h'
        dst = 'o' if si == nsl - 1 else 'h'
        L = plan[layer]
        mk = layer % 3
        c.reset_ps()
        if kind in ('f1', 'f2'):
            sub = FFNSub(c, L[kind + '_in'], L[kind + '_out'], layer, 0 if kind == 'f1' else 4)
        elif mk == 2:
            sub = PoolSub(c, L, layer)
        elif mk == 0:
            sub = HgrnSub(c, L, layer)
        else:
            sub = AttnSub(c, L, layer)
        if kind in ('f1', 'f2'):
            sub.run(tens[src], tens[dst], lambda i, src=src: dtile(src, i), lambda i, dst=dst: dtile(dst, i))
            continue
        for ti in range(NT):
            t0 = ti * T
            sa = tens[src].rearrange("(c p) s -> p c s", p=128)[:, :, t0:t0 + T]
            DMA(c, 'sp', c.H, sa, [dtile(src, 2 * ti), dtile(src, 2 * ti + 1)], [c.Hb])
            sub.tile(ti)
            da = tens[dst].rearrange("(c p) s -> p c s", p=128)[:, :, t0:t0 + T]
            DMA(c, 'sp', da, c.H, [c.Hb], [dtile(dst, 2 * ti), dtile(dst, 2 * ti + 1)])
    P.emit()
    return nc


def MM(c, out, lhsT, rhs, start, stop, reads, writes):
    return c.P.op('pe', lambda e: e.matmul(out, lhsT, rhs, start=start, stop=stop), reads, writes)


def TR(c, out, in_, ident, reads, writes):
    return c.P.op('pe', lambda e: e.transpose(out, in_, ident), reads, writes)


def ACT(c, out, in_, func, reads, writes, bias=0.0, scale=1.0):
    return c.P.op('act', lambda e: e.activation(out=out, in_=in_, func=func, bias=bias, scale=scale), reads, writes)


def TT(c, out, in0, in1, op, reads, writes, eng='dve'):
    return c.P.op(eng, lambda e: e.tensor_tensor(out=out, in0=in0, in1=in1, op=op), reads, writes)


def STT(c, out, in0, scalar, in1, op0, op1, reads, writes, eng='dve'):
    return c.P.op(eng, lambda e: e.scalar_tensor_tensor(out=out, in0=in0, scalar=scalar, in1=in1, op0=op0, op1=op1), reads, writes)


def TS(c, out, in0, s1, s2, op0, op1, reads, writes, eng='dve'):
    if s2 is None:
        return c.P.op(eng, lambda e: e.tensor_scalar(out=out, in0=in0, scalar1=s1, scalar2=None, op0=op0), reads, writes)
    return c.P.op(eng, lambda e: e.tensor_scalar(out=out, in0=in0, scalar1=s1, scalar2=s2, op0=op0, op1=op1), reads, writes)


def CP(c, out, in_, reads, writes, eng='dve'):
    return c.P.op(eng, lambda e: e.tensor_copy(out=out, in_=in_), reads, writes)


def RECIP(c, out, in_, reads, writes):
    return c.P.op('dve', lambda e: e.reciprocal(out=out, in_=in_), reads, writes)


def MEMSET(c, out, val, writes, eng='pool'):
    return c.P.op(eng, lambda e: e.memset(out, val), (), writes)


def DMA(c, eng, out, in_, reads, writes):
    return c.P.op(eng, lambda e: e.dma_start(out=out, in_=in_), reads, writes, dma=True)


def load_slots(c, slots, base, stride_bytes, shape_fn, name):
    out = []
    for i, (off, n) in enumerate(slots):
        lo = base + i * stride_bytes
        assert n * 2 <= stride_bytes
        assert lo + n * 2 <= c.A_W + c.WBYTES, (name, i)
        b = c.R.alloc(lo, n * 2, f"{name}{i}")
        v = c.vb(lo, n)
        DMA(c, 'pool', v, c.wst[:, off:off + n], [], [b])
        out.append((shape_fn(v), b))
    return out


def rstd_from_ps(c, ps, psb, rstd_view, rstd_buf, n=None):
    n = c.T if n is None else n
    ACT(c, rstd_view, ps[:, 0:n], AF.Sqrt, [psb, c.miscb], [rstd_buf], bias=c.epsc)
    RECIP(c, rstd_view, rstd_view, [rstd_buf], [rstd_buf])


def rms_stats(c, sq_view, sq_bufs, nchunks, ones, rstd_view, rstd_buf):
    T = c.T
    ps, psb = c.next_ps()
    for k in range(nchunks):
        MM(c, ps[:, 0:T], ones, sq_view[:, k, :], k == 0, k == nchunks - 1, list(sq_bufs) + [c.miscb], [psb])
    rstd_from_ps(c, ps, psb, rstd_view, rstd_buf)


def prenorm(c, gcol, sq_view, sq_bufs, rstd_view, rstd_buf):
    ACT(c, sq_view, c.H, AF.Square, [c.Hb], list(sq_bufs))
    rms_stats(c, sq_view, sq_bufs, 8, c.ones_d, rstd_view, rstd_buf)
    for k in range(8):
        g = c.cst[:, gcol + k: gcol + k + 1]
        STT(c, c.XN[:, k, :], c.H[:, k, :], g, rstd_view, ALU.mult, ALU.mult, [c.Hb, rstd_buf, c.cstb], [c.XNb])


def postnorm_residual(c, gcol, Y, Yflat, Yb, sq_view, sq_bufs, rstd_view, rstd_buf, factor):
    rms_stats(c, sq_view, sq_bufs, 8, c.ones_d, rstd_view, rstd_buf)
    for k in range(8):
        g = c.cst[:, gcol + k: gcol + k + 1]
        STT(c, Y[:, k, :], Y[:, k, :], g, rstd_view, ALU.mult, ALU.mult, [Yb, rstd_buf, c.cstb], [Yb])
    STT(c, c.Hflat, Yflat, float(factor), c.Hflat, ALU.mult, ALU.add, [Yb, c.Hb], [c.Hb])


class FFNSub:
    def __init__(self, c, slots_in, slots_out, layer, nbase):
        self.c = c
        TF = c.T // 2
        self.TF = TF
        self.gpre = C_GAIN + (layer * 6 + nbase) * 8
        self.gpost = C_GAIN + (layer * 6 + nbase + 1) * 8
        self.win = load_slots(c, slots_in, c.A_W, 8192, lambda v: v.rearrange("p (k n) -> p k n", n=512), 'wi')
        self.wout = load_slots(c, slots_out, c.A_W + 11 * 8192, 5632, lambda v: v.rearrange("p (k n) -> p k n", n=128), 'wo')
        R = c.R
        o = [c.A_H]

        def f32(n, name):
            a = o[0]; o[0] += 4 * n
            return c.vf(a, n), R.alloc(a, 4 * n, name)

        def b16(n, name):
            a = o[0]; o[0] += 2 * n
            return c.vb(a, n), R.alloc(a, 2 * n, name)
        self.H = []; self.Hf = []; self.Hb = []; self.XN = []; self.XNb = []; self.rpre = []; self.rpreb = []
        for i in range(2):
            v, b = f32(8 * TF, f'fH{i}')
            self.Hf.append(v); self.H.append(v.rearrange("p (c t) -> p c t", t=TF)); self.Hb.append(b)
            v, b = b16(8 * TF, f'fXN{i}')
            self.XN.append(v.rearrange("p (c t) -> p c t", t=TF)); self.XNb.append(b)
            v, b = f32(TF, f'frp{i}')
            self.rpre.append(v); self.rpreb.append(b)
        v, b = b16(8 * TF, 'fsq')
        self.sq = v.rearrange("p (c t) -> p c t", t=TF); self.sqb = b
        v, b = f32(8 * TF, 'fY')
        self.Yf = v; self.Y = v.rearrange("p (c t) -> p c t", t=TF); self.Yb = b
        v, b = b16(8 * TF, 'fysq')
        self.ysq = v.rearrange("p (c t) -> p c t", t=TF); self.ysqb = b
        self.rpost, self.rpostb = f32(TF, 'frpost')
        self.sg = []; self.sgb = []
        for i in range(2):
            v, b = f32(TF, f'fsg{i}')
            self.sg.append(v); self.sgb.append(b)
        a = o[0]; o[0] += 2 * NJ * TF
        self.hid = c.vb(a, NJ * TF).rearrange("p (c t) -> p c t", t=TF)
        self.hidb = [R.alloc(a + 2 * TF * j, 2 * TF, f'hid{j}') for j in range(NJ)]
        assert o[0] <= c.A_W, (o[0], c.A_W)

    def run(self, src, dst, sbuf, dbuf):
        c = self.c; TF = self.TF
        N = c.S // TF
        srcv = src.rearrange("(c p) s -> p c s", p=128)
        dstv = dst.rearrange("(c p) s -> p c s", p=128)

        def prologue(i):
            p = i % 2
            H = self.H[p]; Hb = self.Hb[p]; XN = self.XN[p]; XNb = self.XNb[p]
            DMA(c, 'sp', H, srcv[:, :, i * TF:(i + 1) * TF], [sbuf(i)], [Hb])
            ACT(c, self.sq, H, AF.Square, [Hb], [self.sqb])
            ps, psb = c.next_ps()
            for k in range(8):
                MM(c, ps[:, 0:TF], c.ones_d, self.sq[:, k, :], k == 0, k == 7, [self.sqb, c.miscb], [psb])
            rstd_from_ps(c, ps, psb, self.rpre[p], self.rpreb[p], n=TF)
            for k in range(8):
                g = c.cst[:, self.gpre + k: self.gpre + k + 1]
                STT(c, XN[:, k, :], H[:, k, :], g, self.rpre[p], ALU.mult, ALU.mult, [Hb, self.rpreb[p], c.cstb], [XNb])

        def up(i):
            p = i % 2
            XN = self.XN[p]; XNb = self.XNb[p]
            for s in range(11):
                wv, wb = self.win[s]
                for jj in range(2):
                    j = 2 * s + jj
                    psg, psgb = c.next_ps()
                    for k in range(8):
                        MM(c, psg[:, 0:TF], wv[:, k, jj * 128:(jj + 1) * 128], XN[:, k, :], k == 0, k == 7, [wb, XNb], [psgb])
                    psu, psub = c.next_ps()
                    for k in range(8):
                        MM(c, psu[:, 0:TF], wv[:, k, 256 + jj * 128:256 + (jj + 1) * 128], XN[:, k, :], k == 0, k == 7, [wb, XNb], [psub])
                    sg = self.sg[j % 2]; sgb = self.sgb[j % 2]
                    ACT(c, sg, psg[:, 0:TF], AF.Silu, [psgb], [sgb])
                    TT(c, self.hid[:, j, :], sg, psu[:, 0:TF], ALU.mult, [sgb, psub], [self.hidb[j]])

        def down(i):
            for m in range(8):
                wv, wb = self.wout[m]
                ps, psb = c.next_ps()
                for j in range(NJ):
                    MM(c, ps[:, 0:TF], wv[:, j, :], self.hid[:, j, :], j == 0, j == NJ - 1, [wb, self.hidb[j]], [psb])
                ACT(c, self.Y[:, m, :], ps[:, 0:TF], AF.Copy, [psb], [self.Yb])
                ACT(c, self.ysq[:, m, :], ps[:, 0:TF], AF.Square, [psb], [self.ysqb])

        def epilogue(i):
            p = i % 2
            H = self.H[p]; Hb = self.Hb[p]
            ps, psb = c.next_ps()
            for k in range(8):
                MM(c, ps[:, 0:TF], c.ones_d, self.ysq[:, k, :], k == 0, k == 7, [self.ysqb, c.miscb], [psb])
            rstd_from_ps(c, ps, psb, self.rpost, self.rpostb, n=TF)
            for k in range(8):
                g = c.cst[:, self.gpost + k: self.gpost + k + 1]
                STT(c, self.Y[:, k, :], self.Y[:, k, :], g, self.rpost, ALU.mult, ALU.mult, [self.Yb, self.rpostb, c.cstb], [self.Yb])
            STT(c, self.Hf[p], self.Yf, 0.5, self.Hf[p], ALU.mult, ALU.add, [self.Yb, Hb], [Hb])
            DMA(c, 'sp', dstv[:, :, i * TF:(i + 1) * TF], H, [Hb], [dbuf(i)])

        prologue(0)
        for i in range(N):
            up(i)
            if i + 1 < N:
                prologue(i + 1)
            down(i)
            epilogue(i)


def out_proj_residual(c, wout, IN, INbufs, gpost, Y, Yflat, Yb, rstd, rstdb, factor=1.0):
    T = c.T
    for m in range(8):
        wv, wb = wout[m]
        ps, psb = c.next_ps()
        for k in range(8):
            MM(c, ps[:, 0:T], wv[:, k, :], IN[:, k, :], k == 0, k == 7, [wb] + list(INbufs), [psb])
        ACT(c, Y[:, m, :], ps[:, 0:T], AF.Copy, [psb], [Yb])
        ACT(c, c.XN[:, m, :], ps[:, 0:T], AF.Square, [psb], [c.XNb])
    postnorm_residual(c, gpost, Y, Yflat, Yb, c.XN, [c.XNb], rstd, rstdb, factor)


class MixBase:
    def common(self, c, layer):
        T = c.T; R = c.R
        self.c = c
        c.Hb = R.alloc(c.A_H, 32 * T, 'H')
        c.XNb = R.alloc(c.A_XN, 16 * T, 'XN')
        self.gpre = C_GAIN + (layer * 6 + 2) * 8
        self.gpost = C_GAIN + (layer * 6 + 3) * 8
        self.Y = c.vf(c.A_FS, 8 * T).rearrange("p (c t) -> p c t", t=T)
        self.Yflat = c.vf(c.A_FS, 8 * T)
        self.Yb = R.alloc(c.A_FS, 32 * T, 'Y')
        self.rstd = c.vf(c.A_FS + 32 * T, T)
        self.rstdb = R.alloc(c.A_FS + 32 * T, 4 * T, 'rstd')
        self.YV = c.vb(c.A_BS, 8 * T).rearrange("p (c t) -> p c t", t=T)
        self.YVb = [R.alloc(c.A_BS + 2 * T * k, 2 * T, f'YV{k}') for k in range(8)]
        self.SQ = self.YV
        self.SQb = self.YVb


class PoolSub(MixBase):
    def __init__(self, c, L, layer):
        self.common(c, layer)
        T = c.T; R = c.R
        W0 = c.A_W
        self.win = load_slots(c, L['m_in'], W0, 8192, lambda v: v.rearrange("p (k n) -> p k n", n=512), 'pwi')
        self.wgrp = load_slots(c, L['m_grp'], W0 + 16384, 1024, lambda v: v.rearrange("p (k n) -> p k n", n=256), 'pwg')
        self.wout = load_slots(c, L['m_out'], W0 + 20480, 2048, lambda v: v.rearrange("p (k n) -> p k n", n=128), 'pwo')
        E = 16 + T
        self.E = E
        base = W0 + 40960
        self.U = []; self.Ub = []; self.TA = []; self.TAb = []; self.TB = []; self.TBb = []
        for k in range(8):
            for lst, lstb, o in ((self.U, self.Ub, 0), (self.TA, self.TAb, 1), (self.TB, self.TBb, 2)):
                off = base + (o * 8 + k) * 4 * E
                lst.append(c.vf(off, E))
                lstb.append(R.alloc(off, 4 * E, f'pool{o}_{k}'))
        self.PB = c.vb(c.A_BS + 16 * T, 8 * T).rearrange("p (c t) -> p c t", t=T)
        self.PBb = [R.alloc(c.A_BS + 16 * T + 2 * T * k, 2 * T, f'PB{k}') for k in range(8)]
        for k in range(8):
            MEMSET(c, self.U[k][:, 0:16], 0.0, [self.Ub[k]])

    def tile(self, ti):
        c = self.c; T = c.T; E = self.E
        prenorm(c, self.gpre, self.SQ, self.SQb, self.rstd, self.rstdb)
        for m in range(8):
            U = self.U[m]; Ub = self.Ub[m]
            import os
            if ti > 0 and not os.environ.get('NOHALO'):
                CP(c, U[:, 0:16], U[:, T:T + 16], [Ub], [Ub])
            wv, wb = self.win[m // 4]
            ps, psb = c.next_ps()
            for k in range(8):
                MM(c, ps[:, 0:T], wv[:, k, (m % 4) * 128:(m % 4 + 1) * 128], c.XN[:, k, :], k == 0, k == 7, [wb, c.XNb], [psb])
            ACT(c, U[:, 16:E], ps[:, 0:T], AF.Copy, [psb], [Ub])
            g = m // 2
            TA = self.TA[m]; TAb = self.TAb[m]; TB = self.TB[m]; TBb = self.TBb[m]
            TT(c, TA[:, 1:E], U[:, 1:E], U[:, 0:E - 1], ALU.add, [Ub], [TAb])
            Wv, Wb = TA, TAb
            if g >= 1:
                TT(c, TB[:, 3:E], TA[:, 3:E], TA[:, 1:E - 2], ALU.add, [TAb], [TBb])
                Wv, Wb = TB, TBb
            if g >= 2:
                TT(c, TA[:, 7:E], TB[:, 7:E], TB[:, 3:E - 4], ALU.add, [TBb], [TAb])
                Wv, Wb = TA, TAb
            if g >= 3:
                TT(c, TB[:, 15:E], TA[:, 15:E], TA[:, 7:E - 8], ALU.add, [TAb], [TBb])
                Wv, Wb = TB, TBb
            w = 2 ** (g + 1)
            lo = 0
            if ti == 0:
                pinv = c.cst[:, C_PINV + g * 16: C_PINV + (g + 1) * 16]
                TT(c, Wv[:, 16:32], Wv[:, 16:32], pinv, ALU.mult, [Wb, c.cstb], [Wb])
                TT(c, self.PB[:, m, 0:16], Wv[:, 16:32], U[:, 16:32], ALU.subtract, [Wb, Ub], [self.PBb[m]])
                lo = 16
            STT(c, self.PB[:, m, lo:T], Wv[:, 16 + lo:E], 1.0 / w, U[:, 16 + lo:E], ALU.mult, ALU.subtract, [Wb, Ub], [self.PBb[m]])
        for g in range(4):
            wv, wb = self.wgrp[g]
            for oc in range(2):
                mo = 2 * g + oc
                ps, psb = c.next_ps()
                for k2 in range(2):
                    MM(c, ps[:, 0:T], wv[:, k2, oc * 128:(oc + 1) * 128], self.PB[:, 2 * g + k2, :], k2 == 0, k2 == 1,
                       [wb, self.PBb[2 * g + k2]], [psb])
                TS(c, self.YV[:, mo, :], ps[:, 0:T], c.cst[:, C_PSCALE + mo:C_PSCALE + mo + 1], None, ALU.mult, None, [psb, c.cstb], [self.YVb[mo]])
        out_proj_residual(c, self.wout, self.YV, self.YVb, self.gpost, self.Y, self.Yflat, self.Yb, self.rstd, self.rstdb)


class HgrnSub(MixBase):
    def __init__(self, c, L, layer):
        self.common(c, layer)
        T = c.T; R = c.R
        self.layer = layer
        self.j = layer // 3
        W0 = c.A_W
        self.win = load_slots(c, L['m_in'], W0, 8192, lambda v: v.rearrange("p (k n) -> p k n", n=512), 'hwi')
        self.wout = load_slots(c, L['m_out'], W0 + 65536, 2048, lambda v: v.rearrange("p (k n) -> p k n", n=128), 'hwo')
        o = W0 + 81920
        self.St = []; self.Sb = []
        for hd in range(8):
            self.St.append(c.vf(o, 128)); self.Sb.append(R.alloc(o, 512, f'S{hd}')); o += 512
            MEMSET(c, self.St[hd], 0.0, [self.Sb[hd]])
        self.lb = c.vf(o, 8); self.oml = c.vf(o + 32, 8); self.lbE = c.vf(o + 64, 32); self.lbden = c.vf(o + 192, 8)
        self.lbb = R.alloc(o, 256, 'lb'); o += 256
        self.smask = c.vf(o, T); self.smb = R.alloc(o, 4 * T, 'smask'); o += 4 * T
        MEMSET(c, self.smask, 1.0, [self.smb])
        MEMSET(c, self.smask.rearrange("p (b l) -> p b l", l=16)[:, :, 0:1], 0.0, [self.smb])
        raw = c.cst[:, C_LBRAW:C_LBRAW + 32]
        if layer == 0:
            MEMSET(c, self.lb, 0.0, [self.lbb])
        else:
            ACT(c, self.lbE, raw, AF.Exp, [c.cstb], [self.lbb])
            E3 = self.lbE.rearrange("p (c d) -> p c d", d=4)
            c.P.op('dve', lambda e: e.tensor_reduce(out=self.lbden, in_=E3, axis=AX.X, op=ALU.add), [self.lbb], [self.lbb])
            c.P.op('dve', lambda e: e.tensor_reduce(out=self.lb, in_=E3[:, :, 1:layer + 1], axis=AX.X, op=ALU.add), [self.lbb], [self.lbb])
            RECIP(c, self.lbden, self.lbden, [self.lbb], [self.lbb])
            TT(c, self.lb, self.lb, self.lbden, ALU.mult, [self.lbb], [self.lbb])
        TS(c, self.oml, self.lb, -1.0, 1.0, ALU.mult, ALU.add, [self.lbb], [self.lbb])
        G = T // 128
        self.G = G
        self.Vt = c.vb(o, G * 1024).rearrange("p (g n) -> p g n", n=1024)
        self.Vtb = [R.alloc(o + g * 2048, 2048, f'Vt{g}') for g in range(G)]
        o += G * 2048
        free = [[o, c.A_W + c.WBYTES], [c.A_BS + 16 * T, c.A_BS + 44 * T], [c.A_FS + 36 * T, c.A_FS + 48 * T]]

        def take(nbytes):
            for fr in free:
                if fr[1] - fr[0] >= nbytes:
                    a = fr[0]
                    fr[0] += nbytes
                    return a
            raise AssertionError(("hgrn sbuf overflow", nbytes, free))
        self.sets = []
        for par in range(2):
            st = {}
            for nm in ('Q', 'F', 'K', 'B', 'D', 'E', 'QI', 'KS', 'O', 'Gt'):
                a = take(4 * T)
                st[nm] = c.vf(a, T); st[nm + 'b'] = R.alloc(a, 4 * T, f'{nm}{par}')
            for nm in ('QR', 'KR', 'SQh'):
                a = take(2 * T)
                st[nm] = c.vb(a, T); st[nm + 'b'] = R.alloc(a, 2 * T, f'{nm}{par}')
            a = take(max(T // 4, 32))
            st['DEC'] = c.vf(a, T // 16); st['DECb'] = R.alloc(a, max(T // 4, 32), f'DEC{par}')
            a = take(256)
            st['AT'] = c.vb(a, 128); st['ATb'] = R.alloc(a, 256, f'AT{par}')
            a = take(256)
            st['KSt'] = c.vb(a, 128); st['KStb'] = R.alloc(a, 256, f'KSt{par}')
            a = take(2048)
            st['KSm'] = c.vb(a, 1024).rearrange("p (j n) -> p j n", n=128)
            st['KSmb'] = [R.alloc(a + j * 256, 256, f'KSm{par}_{j}') for j in range(8)]
            self.sets.append(st)
        self.gi = 0
        c.ps_lim = 6

    def nq(self):
        ps, psb = self.c.next_ps()
        return ps[:, 0:128], psb

    def tile(self, ti):
        c = self.c; T = c.T; G = self.G
        c.ps_lim = 6
        prenorm(c, self.gpre, self.SQ, self.SQb, self.rstd, self.rstdb)
        for g in range(G):
            for half in range(2):
                wv, wb = self.win[4 + half]
                ps, psb = c.next_ps()
                for k in range(8):
                    MM(c, ps[:, 0:512], c.XN[:, k, g * 128:(g + 1) * 128], wv[:, k, :], k == 0, k == 7, [wb, c.XNb], [psb])
                ACT(c, self.Vt[:, g, half * 512:(half + 1) * 512], ps[:, 0:512], AF.Copy, [psb], [self.Vtb[g]])
        gn = c.cst[:, C_GNORM + self.j:C_GNORM + self.j + 1]
        NB = T // 16

        def prep(hd, st):
            hs = (hd % 4) * 128

            def proj(slot, func, out, outb):
                wv, wb = self.win[slot]
                ps, psb = c.next_ps()
                for k in range(8):
                    MM(c, ps[:, 0:T], wv[:, k, hs:hs + 128], c.XN[:, k, :], k == 0, k == 7, [wb, c.XNb], [psb])
                ACT(c, out, ps[:, 0:T], func, [psb], [outb])
            proj(hd // 4, AF.Copy, st['Q'], st['Qb'])
            proj(2 + hd // 4, AF.Sigmoid, st['F'], st['Fb'])
            proj(6 + hd // 4, AF.Silu, st['Gt'], st['Gtb'])
            TS(c, st['F'], st['F'], self.oml[:, hd:hd + 1], self.lb[:, hd:hd + 1], ALU.mult, ALU.add, [st['Fb'], self.lbb], [st['Fb']])
            ACT(c, st['E'], st['F'], AF.Ln, [st['Fb']], [st['Eb']])
            TS(c, st['K'], st['F'], -1.0, 1.0, ALU.mult, ALU.add, [st['Fb']], [st['Kb']])
            c.P.op('dve', lambda e: e.tensor_tensor_scan(out=st['B'], data0=self.smask, data1=st['E'], initial=0.0,
                                                         op0=ALU.mult, op1=ALU.add),
                   [st['Eb'], self.smb], [st['Bb']])
            B3 = st['B'].rearrange("p (b l) -> p b l", l=16)
            D3 = st['D'].rearrange("p (b l) -> p b l", l=16)
            bmid = B3[:, :, 8:9].broadcast_to([128, NB, 16])
            blast = B3[:, :, 15:16].broadcast_to([128, NB, 16])
            TT(c, D3, B3, bmid, ALU.subtract, [st['Bb']], [st['Db']])
            ACT(c, st['E'], st['D'], AF.Exp, [st['Db']], [st['Eb']])
            TT(c, st['QR'], st['Q'], st['E'], ALU.mult, [st['Qb'], st['Eb']], [st['QRb']])
            ACT(c, st['E'], st['D'], AF.Exp, [st['Db']], [st['Eb']], scale=-1.0)
            TT(c, st['KR'], st['K'], st['E'], ALU.mult, [st['Kb'], st['Eb']], [st['KRb']])
            ACT(c, st['E'], st['B'], AF.Exp, [st['Bb']], [st['Eb']])
            TT(c, st['QI'], st['Q'], st['E'], ALU.mult, [st['Qb'], st['Eb']], [st['QIb']])
            TT(c, D3, blast, B3, ALU.subtract, [st['Bb']], [st['Db']])
            ACT(c, st['E'], st['D'], AF.Exp, [st['Db']], [st['Eb']])
            TT(c, st['KS'], st['K'], st['E'], ALU.mult, [st['Kb'], st['Eb']], [st['KSb']])
            ACT(c, st['DEC'], B3[:, :, 15], AF.Exp, [st['Bb']], [st['DECb']])

        def group_pre(hd, st, g):
            gs = slice(g * 128, (g + 1) * 128)
            vh = self.Vt[:, g, hd * 128:(hd + 1) * 128]
            pa, pab = self.nq()
            MM(c, pa, st['KR'][:, gs], st['QR'][:, gs], True, True, [st['KRb'], st['QRb']], [pab])
            TT(c, st['AT'], pa, c.cst[:, C_HMASK:C_HMASK + 128], ALU.mult, [pab, c.cstb], [st['ATb']])
            po, pob = self.nq()
            MM(c, po, vh, st['AT'], True, True, [self.Vtb[g], st['ATb']], [pob])
            ACT(c, st['O'][:, gs], po, AF.Copy, [pob], [st['Ob']])
            pt, ptb = self.nq()
            TR(c, pt, st['KS'][:, gs], c.identf, [st['KSb'], c.cstb], [ptb])
            for j in range(8):
                ACT(c, st['KSm'][:, j, :], pt, AF.Copy, [ptb, c.cstb], [st['KSmb'][j]], scale=c.cst[:, C_BLKM + j:C_BLKM + j + 1])
            bk = 6 + (hd % 2)
            return c.ps[bk][:, 0:128], c.psb[bk], vh

        def chain(hd, st, g, j, pi, pib, vh):
            Sv = self.St[hd]; Sb = self.Sb[hd]
            blk = g * 8 + j
            MM(c, pi[:, j * 16:(j + 1) * 16], Sv, st['QI'][:, blk * 16:(blk + 1) * 16], True, True, [Sb, st['QIb']], [pib])
            pu, pub = self.nq()
            MM(c, pu, st['KSm'][:, j, :], vh, True, True, [st['KSmb'][j], self.Vtb[g]], [pub])
            STT(c, Sv, Sv, st['DEC'][:, blk:blk + 1], pu, ALU.mult, ALU.add, [Sb, st['DECb'], pub], [Sb])

        def finish(hd, st):
            ACT(c, st['SQh'], st['O'], AF.Square, [st['Ob']], [st['SQhb']])
            ps, psb = c.next_ps()
            MM(c, ps[:, 0:T], c.ones_h, st['SQh'], True, True, [st['SQhb'], c.miscb], [psb])
            rstd_from_ps(c, ps, psb, st['D'], st['Db'])
            STT(c, st['O'], st['O'], gn, st['D'], ALU.mult, ALU.mult, [st['Ob'], st['Db'], c.cstb], [st['Ob']])
            TT(c, self.YV[:, hd, :], st['O'], st['Gt'], ALU.mult, [st['Ob'], st['Gtb']], [self.YVb[hd]])

        for pr in range(4):
            hds = (2 * pr, 2 * pr + 1)
            for hd in hds:
                prep(hd, self.sets[hd % 2])
            for g in range(G):
                gs = slice(g * 128, (g + 1) * 128)
                info = {hd: group_pre(hd, self.sets[hd % 2], g) for hd in hds}
                for j in range(8):
                    for hd in hds:
                        chain(hd, self.sets[hd % 2], g, j, *info[hd])
                for hd in hds:
                    st = self.sets[hd % 2]
                    pi, pib, _ = info[hd]
                    TT(c, st['O'][:, gs], st['O'][:, gs], pi, ALU.add, [pib, st['Ob']], [st['Ob']])
            for hd in hds:
                finish(hd, self.sets[hd % 2])
        out_proj_residual(c, self.wout, self.YV, self.YVb, self.gpost, self.Y, self.Yflat, self.Yb, self.rstd, self.rstdb)


def extra_inputs(S, T):
    NR = T // 128
    tab = np.zeros((128, (NR + 1) * T), np.float32)
    kk = np.arange(128)[:, None].astype(np.float64)
    qq = np.arange(T)[None, :].astype(np.float64)
    tab[:, 0:T] = kk - qq
    for r in range(NR):
        kpos = 128 * r + kk
        allowed = (kpos // 64) <= (qq // 64)
        tab[:, (r + 1) * T:(r + 2) * T] = np.where(allowed, -np.abs(qq - kpos), -1.0e6)
    return {"atab": tab}


class AttnSub(MixBase):
    def __init__(self, c, L, layer):
        self.common(c, layer)
        T = c.T; R = c.R; S = c.S
        self.layer = layer
        W0 = c.A_W
        self.win = load_slots(c, L['m_in'], W0, 8192, lambda v: v.rearrange("p (k n) -> p k n", n=512), 'awi')
        self.wout = load_slots(c, L['m_out'], W0 + 49152, 2048, lambda v: v.rearrange("p (k n) -> p k n", n=128), 'awo')
        o = W0 + 65536
        self.Kb = []; self.Kbb = []; self.Vb = []; self.Vbb = []; self.Kst = []; self.Kstb = []
        for i in range(2):
            self.Kb.append(c.vb(o, S)); self.Kbb.append(R.alloc(o, 2 * S, f'Kb{i}')); o += 2 * S
            self.Vb.append(c.vb(o, S).rearrange("p (t n) -> p t n", n=128)); self.Vbb.append(R.alloc(o, 2 * S, f'Vb{i}')); o += 2 * S
        for i in range(2):
            self.Kst.append(c.vb(o, T)); self.Kstb.append(R.alloc(o, 2 * T, f'Kst{i}')); o += 2 * T
        self.sm = c.vf(o, 80); self.smb = R.alloc(o, 320, 'attn_small'); o += 320
        assert o <= c.A_W + c.WBYTES, o - c.A_W
        lam_init = 0.8 - 0.6 * math.exp(-0.3 * layer)
        lamv = c.cst[:, C_LAM:C_LAM + 256]
        pr = self.sm[:, 0:64]; s1 = self.sm[:, 64:65]; s2 = self.sm[:, 65:66]
        self.nlam = self.sm[:, 66:67]; self.subl = self.sm[:, 67:68]
        TT(c, pr, lamv[:, 0:64], lamv[:, 64:128], ALU.mult, [c.cstb], [self.smb])
        c.P.op('dve', lambda e: e.tensor_reduce(out=s1, in_=pr, axis=AX.X, op=ALU.add), [self.smb], [self.smb])
        TT(c, pr, lamv[:, 128:192], lamv[:, 192:256], ALU.mult, [c.cstb, self.smb], [self.smb])
        c.P.op('dve', lambda e: e.tensor_reduce(out=s2, in_=pr, axis=AX.X, op=ALU.add), [self.smb], [self.smb])
        ACT(c, s1, s1, AF.Exp, [self.smb], [self.smb])
        ACT(c, s2, s2, AF.Exp, [self.smb], [self.smb])
        TT(c, s1, s2, s1, ALU.subtract, [self.smb], [self.smb])
        TS(c, self.nlam, s1, -lam_init, None, ALU.add, None, [self.smb], [self.smb])
        TS(c, self.subl, c.cst[:, C_SUBLN:C_SUBLN + 1], 1.0 - lam_init, None, ALU.mult, None, [c.cstb, self.smb], [self.smb])
        NR = T // 128
        self.NR = NR
        ob = c.A_BS + 16 * T
        self.tab = c.vf(ob, (NR + 1) * T)
        self.tabb = R.alloc(ob, 4 * T * (NR + 1), 'atab')
        DMA(c, 'sp', self.tab, c.atab[:, :], [], [self.tabb])
        ob += 4 * T * (NR + 1)
        self.tmp = []; self.tmpb = []
        for i in range(2):
            self.tmp.append(c.vf(ob, T)); self.tmpb.append(R.alloc(ob, 4 * T, f'atmp{i}')); ob += 4 * T
        assert ob <= c.A_BS + 44 * T, (ob - c.A_BS, 44 * T)
        of = c.A_FS + 36 * T
        self.Qb = []; self.Qbb = []; self.PT = []; self.PTb = []
        for i in range(2):
            self.Qb.append(c.vb(of, T)); self.Qbb.append(R.alloc(of, 2 * T, f'Qb{i}')); of += 2 * T
        for i in range(4):
            self.PT.append(c.vb(of, T)); self.PTb.append(R.alloc(of, 2 * T, f'PT{i}')); of += 2 * T
        assert of <= c.A_FS + 48 * T
        self.ones_k = c.vb(c.A_MISC + 1024, 128)
        MEMSET(c, self.ones_k, 1.0, [c.miscb])
        self.Kd = [Buf(f'ktd{h}') for h in range(8)]
        self.Vd = [Buf(f'vd{h}') for h in range(8)]
        self.pi = 0

    def tile(self, ti):
        c = self.c; T = c.T; S = c.S; NR = self.NR
        G = T // 128
        t0 = ti * T
        c.ps_base = 4; c.ps_lim = 4
        prenorm(c, self.gpre, self.SQ, self.SQb, self.rstd, self.rstdb)
        for hd in range(8):
            wv, wb = self.win[2 + hd // 4]
            hs = (hd % 4) * 128
            ps, psb = c.next_ps()
            for k in range(8):
                MM(c, ps[:, 0:T], wv[:, k, hs:hs + 128], c.XN[:, k, :], k == 0, k == 7, [wb, c.XNb], [psb])
            ks = self.Kst[hd % 2]; ksb = self.Kstb[hd % 2]
            ACT(c, ks, ps[:, 0:T], AF.Copy, [psb], [ksb])
            DMA(c, 'sp', c.ktd[hd, :, t0:t0 + T], ks, [ksb], [self.Kd[hd]])
        Vst = c.vb(c.A_BS, 8 * T).rearrange("p (g n) -> p g n", n=1024)
        for g in range(G):
            for half in range(2):
                wv, wb = self.win[4 + half]
                ps, psb = c.next_ps()
                for k in range(8):
                    MM(c, ps[:, 0:512], c.XN[:, k, g * 128:(g + 1) * 128], wv[:, k, :], k == 0, k == 7, [wb, c.XNb], [psb])
                ACT(c, Vst[:, g, half * 512:(half + 1) * 512], ps[:, 0:512], AF.Copy, [psb], self.YVb)
        for hd in range(8):
            DMA(c, 'sp', c.vd[hd, :, ti * G:(ti + 1) * G, :], Vst[:, :, hd * 128:(hd + 1) * 128], self.YVb, [self.Vd[hd]])
        nk = (ti + 1) * T
        nkt = nk // 128
        scale = 0.125
        for hd in range(8):
            par = hd % 2
            slope = 2.0 ** (-(hd + 1))
            Kb = self.Kb[par]; Kbb = self.Kbb[par]; Vb = self.Vb[par]; Vbb = self.Vbb[par]
            DMA(c, 'sp', Kb[:, 0:nk], c.ktd[hd, :, 0:nk], [self.Kd[hd]], [Kbb])
            DMA(c, 'sp', Vb[:, 0:nkt, :], c.vd[hd, :, 0:nkt, :], [self.Vd[hd]], [Vbb])
            wv, wb = self.win[hd // 4]
            hs = (hd % 4) * 128
            ps, psb = c.next_ps()
            for k in range(8):
                MM(c, ps[:, 0:T], wv[:, k, hs:hs + 128], c.XN[:, k, :], k == 0, k == 7, [wb, c.XNb], [psb])
            Qb = self.Qb[par]; Qbb = self.Qbb[par]
            ACT(c, Qb, ps[:, 0:T], AF.Copy, [psb], [Qbb])
            for g2 in range(2):
                oacc, oaccb = c.ps[2 * g2], c.psb[2 * g2]
                dacc, daccb = c.ps[2 * g2 + 1], c.psb[2 * g2 + 1]
                rows = slice(g2 * 64, (g2 + 1) * 64)
                blocks = []
                for kt in range(nkt):
                    if kt < ti * G:
                        blocks.append((kt, 0, self.tab[:, 0:T], float(t0 - 128 * kt)))
                    else:
                        r = kt - ti * G
                        blocks.append((kt, 128 * r, self.tab[:, (r + 1) * T:(r + 2) * T], 0.0))
                LA = 3
                pend = []
                for n in range(len(blocks) + LA):
                    if n < len(blocks):
                        kt, c0, tb, delta = blocks[n]
                        ps, psb = c.next_ps()
                        MM(c, ps[:, c0:T], Kb[rows, kt * 128:(kt + 1) * 128], Qb[rows, c0:T], True, True, [Kbb, Qbb], [psb])
                        i = self.pi % 4
                        self.pi += 1
                        PT = self.PT[i]; PTb = self.PTb[i]
                        STT(c, ps[:, c0:T], ps[:, c0:T], scale / slope, tb[:, c0:T], ALU.mult, ALU.add, [psb, self.tabb], [psb])
                        ACT(c, PT[:, c0:T], ps[:, c0:T], AF.Exp, [psb], [PTb], bias=float(-slope * delta), scale=slope)
                        pend.append((kt, c0, PT, PTb))
                    if n >= LA:
                        kt, c0, PT, PTb = pend[n - LA]
                        MM(c, oacc[:, c0:T], Vb[:, kt, :], PT[:, c0:T], kt == 0, kt == nkt - 1, [Vbb, PTb], [oaccb])
                        MM(c, dacc[:, c0:T], self.ones_k, PT[:, c0:T], kt == 0, kt == nkt - 1, [c.miscb, PTb], [daccb])
            t0v, t0b, t1v, t1b = self.tmp[0], self.tmpb[0], self.tmp[1], self.tmpb[1]
            RECIP(c, t0v, c.ps[1][:, 0:T], [c.psb[1]], [t0b])
            TT(c, t0v, c.ps[0][:, 0:T], t0v, ALU.mult, [c.psb[0], t0b], [t0b])
            RECIP(c, t1v, c.ps[3][:, 0:T], [c.psb[3]], [t1b])
            TT(c, t1v, c.ps[2][:, 0:T], t1v, ALU.mult, [c.psb[2], t1b], [t1b])
            STT(c, t0v, t1v, self.nlam, t0v, ALU.mult, ALU.add, [t0b, t1b, self.smb], [t0b])
            sq = self.PT[0]; sqb = self.PTb[0]
            ACT(c, sq, t0v, AF.Square, [t0b], [sqb])
            ps, psb = c.next_ps()
            MM(c, ps[:, 0:T], c.ones_h, sq, True, True, [sqb, c.miscb], [psb])
            rstd_from_ps(c, ps, psb, t1v, t1b)
            STT(c, self.YV[:, hd, :], t0v, self.subl, t1v, ALU.mult, ALU.mult, [t0b, t1b, self.smb], [self.YVb[hd]])
        c.ps_base = 0; c.ps_lim = 8
        out_proj_residual(c, self.wout, self.YV, self.YVb, self.gpost, self.Y, self.Yflat, self.Yb, self.rstd, self.rstdb)

    def biasc(self, v):
        return float(v)


SEQ = 8192
TILE = 512
ALL_SUBLAYERS = [(l, k) for l in range(DEPTH) for k in ('f1', 'mix', 'f2')]


def kernel(x, norm_gains, ffn1_w_in, ffn1_w_out, ffn2_w_in, ffn2_w_out,
           hgrn_w_in, hgrn_gnorm, hgrn_w_out, hgrn_lb_raw,
           diff_w_in, diff_lambda, diff_subln, diff_w_out,
           pool_w_in, pool_w_group, pool_scale, pool_w_out):
    inp = dict(x=x, norm_gains=norm_gains, ffn1_w_in=ffn1_w_in, ffn1_w_out=ffn1_w_out,
               ffn2_w_in=ffn2_w_in, ffn2_w_out=ffn2_w_out, hgrn_w_in=hgrn_w_in, hgrn_gnorm=hgrn_gnorm,
               hgrn_w_out=hgrn_w_out, hgrn_lb_raw=hgrn_lb_raw, diff_w_in=diff_w_in, diff_lambda=diff_lambda,
               diff_subln=diff_subln, diff_w_out=diff_w_out, pool_w_in=pool_w_in, pool_w_group=pool_w_group,
               pool_scale=pool_scale, pool_w_out=pool_w_out)
    inp = {k: np.asarray(v, dtype=np.float32) for k, v in inp.items()}
    B, S, _ = inp['x'].shape
    assert S == SEQ and B == 8
    ws, plan = plan_weights(inp)
    wst = ws.array()
    cst = pack_consts(inp)
    ext = extra_inputs(S, TILE)
    nc = build(S, TILE, plan, wst.shape[1], ALL_SUBLAYERS)
    in_maps = []
    for b in range(B):
        in_maps.append({"xT": np.ascontiguousarray(inp['x'][b].T), "wst": wst, "cst": cst, "atab": ext["atab"]})
    res = run_bass_kernel_spmd(nc, in_maps, core_ids=list(range(B)))
    out = np.empty((B, S, D), np.float32)
    for b in range(B):
        out[b] = res.results[b]["oT"].T
    return out
```
